# Optimizing a Trainium2 kernel written in Bass

```python
import math
import jax, jax.numpy as jnp
from jax import lax
import numpy as np

D_MODEL = 2048
BATCH = 16
SEQ = 2048
DEPTH = 1

P_DIM = 256
GRID_W = 64
MIX_WIDTH = D_MODEL
MLSTM_WIDTH = MIX_WIDTH // 2
ATTN_WIDTH = MIX_WIDTH - MLSTM_WIDTH
MLSTM_HEADS = 8
MLSTM_DV = MLSTM_WIDTH // MLSTM_HEADS
MLSTM_DQK = MLSTM_DV // 2
MLSTM_CHUNK = 64
CONV_W = 5
ATTN_HEAD_DIM = 128
ATTN_Q_HEADS = ATTN_WIDTH // ATTN_HEAD_DIM
ATTN_KV_HEADS = 2
ROPE_FREQS = ATTN_HEAD_DIM // 4
ROPE_THETA = 10000.0
Q_BLOCK = 128
N_EXPERTS = 16
EXPERT_FF = D_MODEL // 2
CAPACITY_FACTOR = 2
NORM_EPS = 1e-6
ALPHA = (2.0 * DEPTH) ** 0.25
BETA = (8.0 * DEPTH) ** -0.25

MQ_COLS = MLSTM_HEADS * MLSTM_DQK
MK_COLS = MLSTM_HEADS * MLSTM_DQK
MV_COLS = MLSTM_WIDTH
MO_COLS = MLSTM_WIDTH
MG_COLS = 2 * 2 * MLSTM_HEADS
AQ_COLS = ATTN_Q_HEADS * ATTN_HEAD_DIM
AK_COLS = ATTN_KV_HEADS * ATTN_HEAD_DIM
AV_COLS = ATTN_KV_HEADS * ATTN_HEAD_DIM
IN_SIZES = (MQ_COLS, MK_COLS, MV_COLS, MO_COLS, MG_COLS, AQ_COLS, AK_COLS, AV_COLS)
IN_COLS = MQ_COLS + MK_COLS + MV_COLS + MO_COLS + MG_COLS + AQ_COLS + AK_COLS + AV_COLS

kernel_name = "hybrid_mlstm_axialgqa_ecmoe_deepnorm"


def _split_points():
    pts, acc = [], 0
    for s in IN_SIZES[:-1]:
        acc += s
        pts.append(acc)
    return pts


def _layer_norm(x, g, b):
    xf = x.astype(jnp.float32)
    mu = jnp.mean(xf, axis=-1, keepdims=True)
    var = jnp.mean(jnp.square(xf - mu), axis=-1, keepdims=True)
    return ((xf - mu) * lax.rsqrt(var + NORM_EPS) * g + b).astype(x.dtype)


def _rms_norm(x, g):
    xf = x.astype(jnp.float32)
    return (xf * lax.rsqrt(jnp.mean(xf * xf, axis=-1, keepdims=True) + NORM_EPS) * g).astype(x.dtype)


def _centred_depthwise_conv(x, w):
    c = x.shape[-1]
    return lax.conv_general_dilated(
        x, w[:, None, :], window_strides=(1,), padding=[(CONV_W // 2, CONV_W // 2)],
        dimension_numbers=("NWC", "WIO", "NWC"), feature_group_count=c)


def _mlstm_chunkwise(q, k, v, log_i, log_f):
    B, H, S, _ = q.shape
    dqk, dv = q.shape[-1], v.shape[-1]
    L = MLSTM_CHUNK
    nc = S // L

    def to_chunks(a):
        return jnp.moveaxis(a.reshape(B, H, nc, L, *a.shape[3:]), 2, 0)

    xs = (to_chunks(q), to_chunks(k), to_chunks(v), to_chunks(log_i), to_chunks(log_f))
    lower = jnp.tril(jnp.ones((L, L), dtype=bool))

    def step(carry, inp):
        C, n, m = carry
        qb, kb, vb, li, lf = inp
        b = jnp.cumsum(lf, axis=-1)
        d = jnp.where(lower, b[..., :, None] - b[..., None, :] + li[..., None, :], -jnp.inf)
        inter = b + m[..., None]
        m_t = jnp.maximum(inter, jnp.max(d, axis=-1))
        w_intra = jnp.exp(d - m_t[..., None])
        w_inter = jnp.exp(inter - m_t)
        s = jnp.einsum("bhtd,bhsd->bhts", qb, kb) * w_intra
        num = (jnp.einsum("bhts,bhsv->bhtv", s, vb)
               + w_inter[..., None] * jnp.einsum("bhtd,bhdv->bhtv", qb, C))
        den = jnp.sum(s, axis=-1) + w_inter * jnp.einsum("bhtd,bhd->bht", qb, n)
        h = num / jnp.maximum(jnp.abs(den), jnp.exp(-m_t))[..., None]
        b_last = b[..., -1]
        g = b_last[..., None] - b + li
        m_new = jnp.maximum(b_last + m, jnp.max(g, axis=-1))
        decay = jnp.exp(b_last + m - m_new)
        wk = jnp.exp(g - m_new[..., None])
        C_new = decay[..., None, None] * C + jnp.einsum("bhs,bhsd,bhsv->bhdv", wk, kb, vb)
        n_new = decay[..., None] * n + jnp.einsum("bhs,bhsd->bhd", wk, kb)
        return (C_new, n_new, m_new), h

    init = (jnp.zeros((B, H, dqk, dv), jnp.float32), jnp.zeros((B, H, dqk), jnp.float32),
            jnp.zeros((B, H), jnp.float32))
    _, hs = lax.scan(step, init, xs)
    return jnp.moveaxis(hs, 0, 2).reshape(B, H, S, dv)


def _mlstm_mixer(q, k, v, o, gates, conv_w, b_i, b_f, g_head):
    B, S, _ = q.shape
    qk = jax.nn.silu(_centred_depthwise_conv(jnp.concatenate([q, k], axis=-1), conv_w))
    q, k = jnp.split(qk, 2, axis=-1)

    def heads(a, d):
        return a.reshape(B, S, MLSTM_HEADS, d).transpose(0, 2, 1, 3).astype(jnp.float32)

    qh = heads(q, MLSTM_DQK) * (MLSTM_DQK ** -0.5)
    kh = heads(k, MLSTM_DQK)
    vh = heads(v, MLSTM_DV)
    g = gates.astype(jnp.float32).reshape(B, S, 2, 2, MLSTM_HEADS)
    log_i = (g[:, :, :, 0] + b_i).transpose(2, 0, 3, 1)
    log_f = jax.nn.log_sigmoid(g[:, :, :, 1] + b_f).transpose(2, 0, 3, 1)
    h_fwd = _mlstm_chunkwise(qh, kh, vh, log_i[0], log_f[0])
    rev = lambda a: jnp.flip(a, axis=2)
    h_bwd = rev(_mlstm_chunkwise(rev(qh), rev(kh), rev(vh), rev(log_i[1]), rev(log_f[1])))
    h = _rms_norm((h_fwd + h_bwd).transpose(0, 2, 1, 3), g_head)
    return (jax.nn.sigmoid(o.astype(jnp.float32)) * h.reshape(B, S, MLSTM_WIDTH)).astype(o.dtype)


def _axial_rope_tables(S):
    rows = S // GRID_W
    row_idx = jnp.broadcast_to(jnp.arange(rows, dtype=jnp.float32)[:, None], (rows, GRID_W)).reshape(-1)
    col_idx = jnp.broadcast_to(jnp.arange(GRID_W, dtype=jnp.float32)[None, :], (rows, GRID_W)).reshape(-1)
    inv_freq = ROPE_THETA ** (-jnp.arange(ROPE_FREQS, dtype=jnp.float32) / ROPE_FREQS)
    ang = jnp.stack([row_idx[:, None] * inv_freq, col_idx[:, None] * inv_freq], axis=1)
    return jnp.cos(ang), jnp.sin(ang)


def _apply_axial_rope(x, cos, sin):
    xr = x.astype(jnp.float32).reshape(*x.shape[:-1], 2, 2, ROPE_FREQS)
    x1, x2 = xr[..., 0, :], xr[..., 1, :]
    out = jnp.stack([x1 * cos - x2 * sin, x2 * cos + x1 * sin], axis=-2)
    return out.reshape(x.shape)


def _axial_gqa(q, k, v, g_q, g_k):
    B, S, _ = q.shape
    G = ATTN_Q_HEADS // ATTN_KV_HEADS
    d = ATTN_HEAD_DIM
    qh = q.reshape(B, S, ATTN_KV_HEADS, G, d).transpose(0, 2, 3, 1, 4)
    kh = k.reshape(B, S, ATTN_KV_HEADS, d).transpose(0, 2, 1, 3)
    vh = v.reshape(B, S, ATTN_KV_HEADS, d).transpose(0, 2, 1, 3)
    cos, sin = _axial_rope_tables(S)
    qh = _apply_axial_rope(_rms_norm(qh, g_q), cos, sin) * (d ** -0.5)
    kh = _apply_axial_rope(_rms_norm(kh, g_k), cos, sin)
    nb = S // Q_BLOCK
    qb = qh.reshape(B, ATTN_KV_HEADS, G, nb, Q_BLOCK, d).transpose(3, 0, 1, 2, 4, 5)

    def block(q_blk):
        s = jnp.einsum("bkgqd,bksd->bkgqs", q_blk, kh)
        pr = jax.nn.softmax(s.astype(jnp.float32), axis=-1)
        return jnp.einsum("bkgqs,bksd->bkgqd", pr.astype(vh.dtype), vh)

    ob = lax.map(block, qb)
    return ob.transpose(1, 0, 4, 2, 3, 5).reshape(B, S, ATTN_WIDTH).astype(v.dtype)


def _expert_choice_moe(x, w_router, w_gate, w_up, w_down):
    B, S, D = x.shape
    cap = CAPACITY_FACTOR * S // N_EXPERTS
    affinity = jax.nn.softmax(jnp.einsum("bsd,de->bse", x, w_router).astype(jnp.float32), axis=-1)
    gate, idx = lax.top_k(affinity.transpose(0, 2, 1), cap)
    xs = jax.vmap(lambda xb, ib: xb[ib])(x, idx)
    hid = jax.nn.silu(jnp.einsum("becd,edf->becf", xs, w_gate)) * jnp.einsum("becd,edf->becf", xs, w_up)
    y = jnp.einsum("becf,efd->becd", hid, w_down) * gate[..., None].astype(x.dtype)
    return jax.vmap(lambda yb, ib: jnp.zeros((S, D), yb.dtype).at[ib.reshape(-1)].add(yb.reshape(-1, D)))(y, idx)


def setup_inputs(seed: int = 0) -> dict:
    key = jax.random.key(seed)
    ks = jax.random.split(key, 24)
    f32 = jnp.float32
    nrm = lambda k, shape, s: jax.random.normal(k, shape, f32) * s
    v_scale = jnp.concatenate([
        jnp.ones((MQ_COLS + MK_COLS,), f32), jnp.full((MV_COLS,), BETA, f32),
        jnp.ones((MO_COLS + MG_COLS + AQ_COLS + AK_COLS,), f32), jnp.full((AV_COLS,), BETA, f32)])
    return {
        "x": jax.random.normal(ks[0], (BATCH, SEQ, D_MODEL), f32),
        "p": jax.random.normal(ks[1], (DEPTH, BATCH, SEQ, P_DIM), f32),
        "w_in": nrm(ks[2], (DEPTH, D_MODEL, IN_COLS), D_MODEL ** -0.5) * v_scale,
        "conv_w": nrm(ks[3], (DEPTH, CONV_W, MQ_COLS + MK_COLS), CONV_W ** -0.5),
        "b_igate": nrm(ks[4], (DEPTH, 2, MLSTM_HEADS), 0.1),
        "b_fgate": jnp.broadcast_to(jnp.linspace(3.0, 6.0, MLSTM_HEADS, dtype=f32), (DEPTH, 2, MLSTM_HEADS))
                   + nrm(ks[5], (DEPTH, 2, MLSTM_HEADS), 0.1),
        "g_mlstm": 1.0 + nrm(ks[6], (DEPTH, MLSTM_HEADS, MLSTM_DV), 0.05),
        "g_q": 1.0 + nrm(ks[7], (DEPTH, ATTN_HEAD_DIM), 0.05),
        "g_k": 1.0 + nrm(ks[8], (DEPTH, ATTN_HEAD_DIM), 0.05),
        "w_out": nrm(ks[9], (DEPTH, MIX_WIDTH, D_MODEL), BETA * MIX_WIDTH ** -0.5),
        "ln1_g": 1.0 + nrm(ks[10], (DEPTH, D_MODEL), 0.05),
        "ln1_b": nrm(ks[11], (DEPTH, D_MODEL), 0.02),
        "w_router": nrm(ks[12], (DEPTH, D_MODEL, N_EXPERTS), D_MODEL ** -0.5),
        "w_gate": nrm(ks[13], (DEPTH, N_EXPERTS, D_MODEL, EXPERT_FF), D_MODEL ** -0.5),
        "w_up": nrm(ks[14], (DEPTH, N_EXPERTS, D_MODEL, EXPERT_FF), D_MODEL ** -0.5),
        "w_down": nrm(ks[15], (DEPTH, N_EXPERTS, EXPERT_FF, D_MODEL), BETA * EXPERT_FF ** -0.5),
        "w_pl_proj": nrm(ks[16], (DEPTH, P_DIM, D_MODEL), BETA * P_DIM ** -0.5),
        "w_pl_gate": nrm(ks[17], (DEPTH, D_MODEL, D_MODEL), D_MODEL ** -0.5),
        "b_pl_gate": nrm(ks[18], (DEPTH, D_MODEL), 0.02),
        "ln2_g": 1.0 + nrm(ks[19], (DEPTH, D_MODEL), 0.05),
        "ln2_b": nrm(ks[20], (DEPTH, D_MODEL), 0.02),
    }


def reference(x, p, w_in, conv_w, b_igate, b_fgate, g_mlstm, g_q, g_k, w_out, ln1_g, ln1_b,
              w_router, w_gate, w_up, w_down, w_pl_proj, w_pl_gate, b_pl_gate, ln2_g, ln2_b):
    splits = _split_points()
    for i in range(DEPTH):
        proj = jnp.einsum("bsd,dc->bsc", x, w_in[i])
        mq, mk, mv, mo, mg, aq, ak, av = jnp.split(proj, splits, axis=-1)
        h_mlstm = _mlstm_mixer(mq, mk, mv, mo, mg, conv_w[i], b_igate[i], b_fgate[i], g_mlstm[i])
        h_attn = _axial_gqa(aq, ak, av, g_q[i], g_k[i])
        mix = jnp.einsum("bsc,cd->bsd", jnp.concatenate([h_mlstm, h_attn], axis=-1), w_out[i])
        x = _layer_norm(ALPHA * x + mix, ln1_g[i], ln1_b[i])
        moe = _expert_choice_moe(x, w_router[i], w_gate[i], w_up[i], w_down[i])
        pl_gate = jax.nn.sigmoid(jnp.einsum("bsd,de->bse", x, w_pl_gate[i]) + b_pl_gate[i])
        pl = pl_gate * jnp.einsum("bsp,pd->bsd", p[i], w_pl_proj[i])
        x = _layer_norm(ALPHA * x + moe + pl, ln2_g[i], ln2_b[i])
    return x
```

```python
import math
from contextlib import ExitStack
import numpy as np
import concourse.bass as bass
import concourse.mybir as mybir
from concourse.alu_op_type import AluOpType as ALU
from concourse.bass_utils import run_bass_kernel_spmd

AF = mybir.ActivationFunctionType
F32 = mybir.dt.float32
BF16 = mybir.dt.bfloat16
I32 = mybir.dt.int32
U32 = mybir.dt.uint32

ENGS = ("pe", "act", "dve", "pool", "sp")
D = 2048
S = 2048
NT = 16
NEXP = 16
CAP = 256
FF = 1024
EPS = 1e-6
ALPHA = 2.0 ** 0.25


class T:
    __slots__ = ("name", "w", "r")

    def __init__(self, name):
        self.name = name
        self.w = []
        self.r = []


class Op:
    __slots__ = ("idx", "eng", "fn", "deps", "dma", "flag", "evt")

    def __init__(self, idx, eng, fn, deps, dma):
        self.idx = idx
        self.eng = eng
        self.fn = fn
        self.deps = deps
        self.dma = dma
        self.flag = False
        self.evt = None


class Prog:
    def __init__(self, nc, n_dma_sems=32):
        self.nc = nc
        self.ops = []
        self.q = {e: [] for e in ENGS}
        self.tiles = []
        self.nd = n_dma_sems
        self.esem = {e: nc.alloc_semaphore("s_" + e) for e in ENGS}
        self.dsems = [nc.alloc_semaphore("d%d" % i) for i in range(n_dma_sems)]
        self.dval = [0] * n_dma_sems
        self.dnext = {"sp": 0, "pool": 0}
        self.cnt = {e: 0 for e in ENGS}
        self.di = 0
        self.known = {e: {} for e in ENGS}
        self.nins = 0

    def T(self, name):
        t = T(name)
        self.tiles.append(t)
        return t

    def Ts(self, name, n):
        return [self.T("%s%d" % (name, i)) for i in range(n)]

    def op(self, eng, fn, reads=(), writes=(), dma=False, partial=False):
        idx = len(self.ops)
        deps = set()
        for t in reads:
            deps.update(t.w)
        for t in writes:
            deps.update(t.w)
            deps.update(t.r)
        o = Op(idx, eng, fn, deps, dma)
        for t in reads:
            t.r.append(idx)
        for t in writes:
            if partial:
                t.w.append(idx)
            else:
                t.w = [idx]
            t.r = []
        self.ops.append(o)
        self.q[eng].append(o)
        return o

    def flush(self, final=False):
        nc = self.nc
        ops = self.ops
        alld = set(o.idx for o in ops if o.dma)
        for e in ENGS:
            if self.q[e]:
                alld.add(self.q[e][-1].idx)
        for e in ENGS:
            o = Op(len(ops), e, lambda eng: eng.nop(), set(alld), False)
            ops.append(o)
            self.q[e].append(o)
        for o in ops:
            best = {}
            keep = set()
            for d in o.deps:
                p = ops[d]
                if p.dma:
                    keep.add(d)
                    continue
                if p.eng == "pe" and o.eng == "pe" and not o.dma:
                    continue
                b = best.get(p.eng)
                if b is None or d > b:
                    best[p.eng] = d
            keep.update(best.values())
            o.deps = keep
            for d in keep:
                ops[d].flag = True
        dma_prev = {}
        for o in ops:
            if o.dma:
                half = self.nd // 2
                s = (self.dnext[o.eng] % half) + (0 if o.eng == "sp" else half)
                self.dnext[o.eng] += 1
                dma_prev[o.idx] = (self.dsems[s], self.dval[s])
                self.dval[s] += 16
                o.evt = (self.dsems[s], self.dval[s])
            elif o.flag:
                self.cnt[o.eng] += 1
                o.evt = (self.esem[o.eng], self.cnt[o.eng])
        finals = [(self.dsems[i], self.dval[i]) for i in range(self.nd) if self.dval[i]]

        def run_queue(ename, eng):
            known = self.known[ename]
            for o in self.q[ename]:
                need = {}
                ws = []
                if o.dma:
                    ws.append(dma_prev[o.idx])
                for d in o.deps:
                    ws.append(ops[d].evt)
                for (s, v) in ws:
                    if v <= 0:
                        continue
                    k = id(s)
                    if known.get(k, 0) >= v:
                        continue
                    if k not in need or need[k][1] < v:
                        need[k] = (s, v)
                for k, (s, v) in need.items():
                    known[k] = v
                    eng.wait_ge(s, v)
                    self.nins += 1
                ins = o.fn(eng)
                self.nins += 1
                if o.dma:
                    ins.then_inc(o.evt[0], 16)
                elif o.flag:
                    ins.then_inc(o.evt[0], 1)
            if final and ename == "sp":
                for (s, v) in finals:
                    eng.wait_ge(s, v)

        with nc.Block() as block:
            @block.tensor
            def _(e):
                run_queue("pe", e)

            @block.scalar
            def _(e):
                run_queue("act", e)

            @block.vector
            def _(e):
                run_queue("dve", e)

            @block.gpsimd
            def _(e):
                run_queue("pool", e)

            @block.sync
            def _(e):
                run_queue("sp", e)
        self.ops = []
        self.q = {e: [] for e in ENGS}
        for t in self.tiles:
            t.w = []
            t.r = []


class Ring:
    def __init__(self, items):
        self.items = items
        self.i = 0

    def next(self):
        it = self.items[self.i % len(self.items)]
        self.i += 1
        return it


def build(nseq=2, stages="ABCMF", dbg=False):
    import os
    SKIP = set(os.environ.get("KSKIP", "").split(","))
    nc = bass.Bass("TRN2", target_bir_lowering=False)
    NTOK = nseq * S
    P = Prog(nc)

    def din(name, shape, dt=F32):
        return nc.dram_tensor(name, list(shape), dt, kind="ExternalInput").ap()

    x_d = din("x", [NTOK, D])
    p_d = din("p", [NTOK, 256])
    w_in = din("w_in", [D, 4640])
    convT = din("convT", [1024, 5])
    gbias_d = din("gbias", [1, 32])
    gml_d = din("g_mlstm", [1, 1024])
    gq_d = din("g_q", [128, 1])
    gk_d = din("g_k", [128, 1])
    w_out = din("w_out", [D, D])
    ln1g_d = din("ln1_g", [1, D])
    ln1b_d = din("ln1_b", [1, D])
    wr_d = din("w_router", [D, NEXP])
    wg_d = din("w_gate", [NEXP, D, FF])
    wu_d = din("w_up", [NEXP, D, FF])
    wd_d = din("w_down", [NEXP, FF, D])
    wpp_d = din("w_pl_proj", [256, D])
    wpg_d = din("w_pl_gate", [D, D])
    bpg_d = din("b_pl_gate", [1, D])
    ln2g_d = din("ln2_g", [1, D])
    ln2b_d = din("ln2_b", [1, D])
    cst_d = din("cst", [6, 128, 128])
    cos_d = din("cosT", [128, S])
    sin_d = din("sinT", [128, S])
    out_d = nc.dram_tensor("out", [NTOK, D], F32, kind="ExternalOutput").ap()
    ikind = "ExternalOutput" if dbg else "Internal"
    HT = nc.dram_tensor("HT", [nseq, D, S], BF16, kind=ikind).ap()
    X1B = nc.dram_tensor("X1B", [NTOK, D], BF16, kind=ikind).ap()
    R = nc.dram_tensor("R", [NTOK, D], F32, kind=ikind).ap()
    dbg_out = {}
    _uq = [0]

    def uq_name(n):
        _uq[0] += 1
        return "%s_u%d" % (n, _uq[0])

    def dout(name, shape, dt=F32):
        a = nc.dram_tensor(name, list(shape), dt, kind="ExternalOutput").ap()
        dbg_out[name] = a
        return a

    cf = nc.alloc_sbuf_tensor("cf", [128, 6, 128], F32)
    cb = nc.alloc_sbuf_tensor("cb", [128, 6, 128], BF16)
    onesb = nc.alloc_sbuf_tensor("onesb", [128, 128], BF16)
    t_c = P.T("consts")
    IDENT, MASKF, MASKB, SELL, SELF, PERM = range(6)
    P.op("sp", lambda e: e.dma_start(out=cf[:], in_=cst_d.rearrange("c p f -> p c f")), writes=[t_c], dma=True)
    P.op("dve", lambda e: e.tensor_copy(out=cb[:], in_=cf[:]), reads=[t_c], writes=[t_c], partial=True)
    P.op("dve", lambda e: e.memset(onesb[:], 1.0), writes=[t_c], partial=True)

    banks = [nc.alloc_psum_tensor("bank%d" % i, [128, 512], F32) for i in range(8)]
    tb = P.Ts("bank", 8)

    def bank_bf(i):
        return banks[i][:].bitcast(BF16)

    def dma(q, out, in_, reads=(), writes=(), partial=False):
        return P.op(q, lambda e: e.dma_start(out=out, in_=in_), reads=reads, writes=writes, dma=True, partial=partial)

    def mm(out, lhsT, rhs, start, stop, reads, writes):
        return P.op("pe", lambda e: e.matmul(out, lhsT, rhs, start=start, stop=stop), reads=reads, writes=writes,
                    partial=not start)

    def act(out, in_, func, reads, writes, bias=None, scale=None, accum_out=None, partial=False):
        kw = {}
        if bias is not None:
            kw["bias"] = bias
        if scale is not None:
            kw["scale"] = scale
        if accum_out is not None:
            kw["accum_out"] = accum_out
        return P.op("act", lambda e: e.activation(out, in_, func, **kw), reads=reads, writes=writes, partial=partial)

    def ts(eng, out, in0, s1, op0, reads, writes, s2=None, op1=None, partial=False):
        if op1 is None:
            return P.op(eng, lambda e: e.tensor_scalar(out, in0, s1, None, op0), reads=reads, writes=writes, partial=partial)
        return P.op(eng, lambda e: e.tensor_scalar(out, in0, s1, s2, op0, op1), reads=reads, writes=writes, partial=partial)

    def tt(eng, out, in0, in1, op, reads, writes, partial=False):
        return P.op(eng, lambda e: e.tensor_tensor(out, in0, in1, op), reads=reads, writes=writes, partial=partial)

    def stt(out, in0, scalar, in1, op0, op1, reads, writes, partial=False):
        return P.op("dve", lambda e: e.scalar_tensor_tensor(out, in0, scalar, in1, op0, op1), reads=reads, writes=writes,
                    partial=partial)

    def cp(eng, out, in_, reads, writes, partial=False):
        if eng == "act":
            return P.op("act", lambda e: e.copy(out, in_), reads=reads, writes=writes, partial=partial)
        return P.op(eng, lambda e: e.tensor_copy(out=out, in_=in_), reads=reads, writes=writes, partial=partial)

    def recip(out, in_, reads, writes):
        return P.op("dve", lambda e: e.reciprocal(out, in_), reads=reads, writes=writes)

    def load_w(slot, tslot, pieces):
        first = True
        for (c0, ap) in pieces:
            n = ap.shape[1]
            kc = ap.shape[0] // 128
            dma("pool", slot[:, 0:kc, c0:c0 + n], ap.rearrange("(c p) f -> p c f", p=128), writes=[tslot],
                partial=not first)
            first = False

    def bcast_row(dst, row_ap, tdst):
        dma("sp", dst, row_ap.partition_broadcast(128), writes=[tdst])

    affT = nc.alloc_sbuf_tensor("affT", [64, S], F32)
    t_affT = P.T("affT")
    P.op("pool", lambda e: e.memset(affT[:], 0.0), writes=[t_affT])

    def layernorm(x, t_x, g, b, t_gb, w, t_w):
        st = w[:, 0:24].rearrange("p (a b) -> p a b", a=4)
        for c in range(4):
            P.op("dve", lambda e, c=c: e.bn_stats(out=st[:, c, :], in_=x[:, c * 512:(c + 1) * 512]), reads=[t_x], writes=[t_w],
                 partial=c > 0)
        P.op("dve", lambda e: e.bn_aggr(out=w[:, 24:26], in_=w[:, 0:24]), reads=[t_w], writes=[t_w])
        act(w[:, 26:27], w[:, 25:26], AF.Sqrt, reads=[t_w], writes=[t_w], bias=EPS, scale=1.0)
        recip(w[:, 26:27], w[:, 26:27], reads=[t_w], writes=[t_w])
        ts("dve", x[:], x[:], w[:, 24:25], ALU.subtract, reads=[t_x, t_w], writes=[t_x], s2=w[:, 26:27], op1=ALU.mult)
        tt("pool", x[:], x[:], g[:], ALU.mult, reads=[t_x, t_gb], writes=[t_x])
        tt("pool", x[:], x[:], b[:], ALU.add, reads=[t_x, t_gb], writes=[t_x])

    P.flush()

    for seq in range(nseq):
        tok0 = seq * S
        with ExitStack() as seq_es:
            if any(c in stages for c in "AB"):
                xT = seq_es.enter_context(nc.sbuf_tensor(uq_name("xT"), [128, 16, S], BF16))
                t_xT = P.Ts("xT", NT)
                with ExitStack() as es:
                    xin = [es.enter_context(nc.sbuf_tensor(uq_name("xin%d" % i), [128, D], F32)) for i in range(2)]
                    t_xin = P.Ts("xin", 2)
                    k = 0
                    for t in range(NT):
                        b = t % 2
                        dma("sp", xin[b][:], x_d[tok0 + t * 128: tok0 + (t + 1) * 128, :], writes=[t_xin[b]])
                        for g in range(4):
                            bi = k % 4
                            k += 1
                            for j in range(4):
                                c = g * 4 + j
                                P.op("pe", lambda e, bi=bi, j=j, b=b, c=c: e.transpose(
                                    banks[bi][:, j * 128:(j + 1) * 128], xin[b][:, c * 128:(c + 1) * 128], cf[:, IDENT, :]),
                                    reads=[t_xin[b], t_c], writes=[tb[bi]], partial=j > 0)
                            cp("act" if g % 2 == 0 else "dve", xT[:, g * 4:(g + 1) * 4, t * 128:(t + 1) * 128],
                               banks[bi][:].rearrange("p (a b) -> p a b", a=4), reads=[tb[bi]], writes=[t_xT[t]],
                               partial=g > 0)
                    P.flush()

            if "A" in stages:
                with ExitStack() as es:
                    al = lambda name, shape, dt: es.enter_context(nc.sbuf_tensor(uq_name(name), shape, dt))
                    qT = al("qT", [128, 8, S], BF16)
                    kT = al("kT", [128, 2, S], BF16)
                    vtok = al("vtok", [128, NT, 256], BF16)
                    cosS = al("cosS", [128, S], F32)
                    sinS = al("sinS", [128, S], F32)
                    gqk = al("gqk", [128, 2], F32)
                    es2 = ExitStack()
                    al2 = lambda name, shape, dt: es2.enter_context(nc.sbuf_tensor(uq_name(name), shape, dt))
                    wsl = [al2("wa%d" % i, [128, 16, 512], BF16) for i in range(3)]
                    t_wsl = P.Ts("wa", 3)
                    wring = Ring(list(zip(wsl, t_wsl)))
                    t_qT = [[P.T("qT%d_%d" % (h, b)) for b in range(4)] for h in range(8)]
                    t_kT = [[P.T("kT%d_%d" % (h, b)) for b in range(4)] for h in range(2)]
                    t_v = P.Ts("vtok", NT)
                    t_tab = P.T("tab")
                    dma("sp", cosS[:], cos_d, writes=[t_tab])
                    dma("sp", sinS[:], sin_d, writes=[t_tab], partial=True)
                    dma("sp", gqk[:, 0:1], gq_d, writes=[t_tab], partial=True)
                    dma("sp", gqk[:, 1:2], gk_d, writes=[t_tab], partial=True)
                    ts("dve", gqk[:, 0:1], gqk[:, 0:1], 128.0 ** -0.5, ALU.mult, reads=[t_tab], writes=[t_tab], partial=True)
                    nw = 2
                    qf = [al2("qf%d" % i, [128, 512], F32) for i in range(nw)]
                    sq = [al2("sq%d" % i, [128, 512], BF16) for i in range(nw)]
                    sd = [al2("sd%d" % i, [128, 512], F32) for i in range(nw)]
                    qn = [al2("qn%d" % i, [128, 512], BF16) for i in range(nw)]
                    t1 = [al2("t1%d" % i, [128, 512], F32) for i in range(1)] * nw
                    t2 = [al2("t2%d" % i, [128, 512], F32) for i in range(1)] * nw
                    t_qf, t_sq, t_sd, t_qn = (P.Ts(n, nw) for n in ("qf", "sq", "sd", "qn"))
                    t_t1 = P.Ts("t1", 1) * nw
                    t_t2 = P.Ts("t2", 1) * nw
                    nrc = [0]
                    accring = Ring([0, 1])
                    auxring = Ring([2, 3])

                    def normrope(bi, out_ap, t_out, gcol, tblk):
                        i = nrc[0] % nw
                        nrc[0] += 1
                        ps = banks[bi]
                        cp("act", qf[i][:], ps[:], reads=[tb[bi]], writes=[t_qf[i]])
                        act(sq[i][:], ps[:], AF.Square, reads=[tb[bi]], writes=[t_sq[i]])
                        b2 = auxring.next()
                        mm(banks[b2][:], onesb[:], sq[i][:], True, True, reads=[t_sq[i], t_c], writes=[tb[b2]])
                        act(sd[i][:], banks[b2][:], AF.Sqrt, reads=[tb[b2]], writes=[t_sd[i]], bias=EPS, scale=1.0 / 128)
                        recip(sd[i][:], sd[i][:], reads=[t_sd[i]], writes=[t_sd[i]])
                        stt(qn[i][:], qf[i][:], gqk[:, gcol:gcol + 1], sd[i][:], ALU.mult, ALU.mult,
                            reads=[t_qf[i], t_sd[i], t_tab], writes=[t_qn[i]])
                        b3 = auxring.next()
                        mm(banks[b3][:], cb[:, PERM, :], qn[i][:], True, True, reads=[t_qn[i], t_c], writes=[tb[b3]])
                        cs = slice(tblk * 512, (tblk + 1) * 512)
                        tt("pool", t1[i][:], qn[i][:], cosS[:, cs], ALU.mult, reads=[t_qn[i], t_tab], writes=[t_t1[i]])
                        tt("dve", t2[i][:], banks[b3][:], sinS[:, cs], ALU.mult, reads=[tb[b3], t_tab], writes=[t_t2[i]])
                        tt("pool", out_ap, t1[i][:], t2[i][:], ALU.add, reads=[t_t1[i], t_t2[i]], writes=[t_out])

                    def fm_block(w, tw, c0, tblk, bi):
                        for k in range(16):
                            mm(banks[bi][:], w[:, k, c0:c0 + 128], xT[:, k, tblk * 512:(tblk + 1) * 512], k == 0, k == 15,
                               reads=[tw] + t_xT[tblk * 4:(tblk + 1) * 4], writes=[tb[bi]])

                    w, tw = wring.next()
                    load_w(w, tw, [(0, w_in[:, 4128:4640])])
                    w2, tw2 = wring.next()
                    load_w(w2, tw2, [(0, w_in[:, 3104:3616])])
                    w3, tw3 = wring.next()
                    load_w(w3, tw3, [(0, w_in[:, 3616:4128])])
                    for h in range(2):
                        for blk in range(4):
                            bi = accring.next()
                            fm_block(w, tw, h * 128, blk, bi)
                            normrope(bi, kT[:, h, blk * 512:(blk + 1) * 512], t_kT[h][blk], 1, blk)
                    for t in range(NT):
                        bi = accring.next()
                        for k in range(16):
                            mm(banks[bi][:, 0:256], xT[:, k, t * 128:(t + 1) * 128], w[:, k, 256:512], k == 0, k == 15,
                               reads=[tw, t_xT[t]], writes=[tb[bi]])
                        cp("act" if t % 2 else "dve", vtok[:, t, :], banks[bi][:, 0:256], reads=[tb[bi]], writes=[t_v[t]])
                    for half, (wq, twq) in enumerate(((w2, tw2), (w3, tw3))):
                        for hh in range(4):
                            h = half * 4 + hh
                            for blk in range(4):
                                bi = accring.next()
                                fm_block(wq, twq, hh * 128, blk, bi)
                                normrope(bi, qT[:, h, blk * 512:(blk + 1) * 512], t_qT[h][blk], 0, blk)
                    if dbg and seq == 0:
                        d_q = dout("dbg_qT", [128, 8, S], BF16)
                        d_k = dout("dbg_kT", [128, 2, S], BF16)
                        d_v = dout("dbg_v", [128, NT, 256], BF16)
                        dma("sp", d_q, qT[:], reads=[t for r in t_qT for t in r])
                        dma("sp", d_k, kT[:], reads=[t for r in t_kT for t in r])
                        dma("sp", d_v, vtok[:], reads=t_v)
                    P.flush()
                    es2.close()
                    NPT = 6
                    pT = [al("pT%d" % i, [128, 512], BF16) for i in range(NPT)]
                    t_pT = P.Ts("pT", NPT)
                    rden = [al("rden%d" % i, [128, 512], F32) for i in range(2)]
                    t_rden = P.Ts("rden", 2)
                    ost = [al("ost%d" % i, [128, 512], BF16) for i in range(2)]
                    t_ost = P.Ts("ost", 2)
                    scring = Ring([0, 1, 2, 3])
                    oring = Ring([(4, 5), (6, 7)])
                    pring = Ring(list(range(NPT)))
                    PF = 3
                    t_HT = P.T("HT")
                    it = 0
                    for h in range(8):
                        g = h // 4
                        for qb in range(4):
                            bo, bd = oring.next()
                            qs = slice(qb * 512, (qb + 1) * 512)

                            def sc(kt):
                                bs = scring.next()
                                mm(banks[bs][:], kT[:, g, kt * 128:(kt + 1) * 128], qT[:, h, qs], True, True,
                                   reads=[t_kT[g][kt // 4], t_qT[h][qb]], writes=[tb[bs]])
                                return bs
                            pend = [sc(kt) for kt in range(PF)]
                            for kt in range(NT):
                                bs = pend.pop(0)
                                pi = pring.next()
                                act(pT[pi][:], banks[bs][:], AF.Exp, reads=[tb[bs]], writes=[t_pT[pi]])
                                if kt + PF < NT:
                                    pend.append(sc(kt + PF))
                                mm(banks[bo][:], vtok[:, kt, g * 128:(g + 1) * 128], pT[pi][:], kt == 0, kt == NT - 1,
                                   reads=[t_v[kt], t_pT[pi]], writes=[tb[bo]])
                                mm(banks[bd][:], onesb[:], pT[pi][:], kt == 0, kt == NT - 1,
                                   reads=[t_c, t_pT[pi]], writes=[tb[bd]])
                            i2 = it % 2
                            it += 1
                            recip(rden[i2][:], banks[bd][:], reads=[tb[bd]], writes=[t_rden[i2]])
                            tt("dve", ost[i2][:], banks[bo][:], rden[i2][:], ALU.mult, reads=[tb[bo], t_rden[i2]],
                               writes=[t_ost[i2]])
                            dma("sp", HT[seq, 1024 + h * 128: 1024 + (h + 1) * 128, qs], ost[i2][:], reads=[t_ost[i2]],
                                writes=[t_HT], partial=True)
                    P.flush()

            if "B" in stages:
                for G in range(2):
                    with ExitStack() as es:
                        al = lambda name, shape, dt: es.enter_context(nc.sbuf_tensor(uq_name(name), shape, dt))
                        qkT = al("qkT", [128, 4, S], BF16)
                        t_qk = [[P.T("qk%d_%d" % (c, t)) for t in range(4)] for c in range(4)]
                        ktok = al("ktok", [128, NT, 256], BF16)
                        t_ktok = P.Ts("ktok", NT)
                        vbase = al("vbase", [128, NT, 4, 129], BF16)
                        t_vb = P.Ts("vb", NT)
                        so = al("so", [128, NT, 512], BF16)
                        t_so = P.Ts("so", NT)
                        gsm = al("gsm", [128, NT, 4, 16], F32)
                        t_gs = P.Ts("gs", NT)
                        cw = al("cw", [128, 4, 5], F32)
                        gbias = al("gbias_sb", [128, 32], F32)
                        gml = al("gml", [128, 512], F32)
                        es2 = ExitStack()
                        al2 = lambda name, shape, dt: es2.enter_context(nc.sbuf_tensor(uq_name(name), shape, dt))
                        wsl = [al2("wb%d" % i, [128, 16, 512], BF16) for i in range(3)]
                        t_wsl = P.Ts("wb", 3)
                        wgt = al2("wgt", [128, 16, 32], BF16)
                        t_wgt = P.T("wgt")
                        t_misc = P.T("miscB")
                        dma("sp", cw[:, 0:2, :], convT[G * 256:(G + 1) * 256, :].rearrange("(c p) k -> p c k", p=128),
                            writes=[t_misc])
                        dma("sp", cw[:, 2:4, :], convT[512 + G * 256:512 + (G + 1) * 256, :].rearrange("(c p) k -> p c k", p=128),
                            writes=[t_misc], partial=True)
                        bcast_row(gbias[:], gbias_d, t_misc)
                        dma("sp", gml[:], gml_d[:, G * 512:(G + 1) * 512].partition_broadcast(128), writes=[t_misc], partial=True)
                        P.op("pool", lambda e: e.memset(vbase[:, :, :, 128:129], 1.0), writes=t_vb)
                        load_w(wsl[0], t_wsl[0], [(0, w_in[:, G * 256:(G + 1) * 256]), (256, w_in[:, 512 + G * 256:512 + (G + 1) * 256])])
                        load_w(wsl[1], t_wsl[1], [(0, w_in[:, 1024 + G * 512:1024 + (G + 1) * 512])])
                        load_w(wsl[2], t_wsl[2], [(0, w_in[:, 2048 + G * 512:2048 + (G + 1) * 512])])
                        load_w(wgt, t_wgt, [(0, w_in[:, 3072:3104])])
                        pc = [al2("pc%d" % i, [128, S + 4], BF16) for i in range(2)]
                        t_pc = P.Ts("pc", 2)
                        cacc = [al2("cacc%d" % i, [128, S], F32) for i in range(1)] * 2
                        t_cacc = P.Ts("cacc", 1) * 2
                        for i in range(2):
                            P.op("pool", lambda e, i=i: e.memset(pc[i][:, 0:2], 0.0), writes=[t_pc[i]])
                            P.op("pool", lambda e, i=i: e.memset(pc[i][:, S + 2:S + 4], 0.0), writes=[t_pc[i]], partial=True)
                        accring = Ring([0, 1, 2, 3])
                        for c in range(4):
                            i = c % 2
                            for blk in range(4):
                                bi = accring.next()
                                for k in range(16):
                                    mm(banks[bi][:], wsl[0][:, k, c * 128:(c + 1) * 128], xT[:, k, blk * 512:(blk + 1) * 512],
                                       k == 0, k == 15, reads=[t_wsl[0]] + t_xT[blk * 4:(blk + 1) * 4], writes=[tb[bi]])
                                cp("act", pc[i][:, 2 + blk * 512: 2 + (blk + 1) * 512], banks[bi][:], reads=[tb[bi]],
                                   writes=[t_pc[i]], partial=True)
                            ts("dve", cacc[i][:], pc[i][:, 0:S], cw[:, c, 0:1], ALU.mult, reads=[t_pc[i], t_misc], writes=[t_cacc[i]])
                            for j in range(1, 5):
                                stt(cacc[i][:], pc[i][:, j:j + S], cw[:, c, j:j + 1], cacc[i][:], ALU.mult, ALU.add,
                                    reads=[t_pc[i], t_misc, t_cacc[i]], writes=[t_cacc[i]])
                            for blk in range(4):
                                act(qkT[:, c, blk * 512:(blk + 1) * 512], cacc[i][:, blk * 512:(blk + 1) * 512], AF.Silu,
                                    reads=[t_cacc[i]], writes=[t_qk[c][blk]])
                        for t in range(NT):
                            bi = accring.next()
                            for pr in range(2):
                                P.op("pe", lambda e, bi=bi, pr=pr, t=t: e.transpose(
                                    bank_bf(bi)[:, pr * 128:(pr + 1) * 128], qkT[:, 2 + pr, t * 128:(t + 1) * 128], cb[:, IDENT, :]),
                                    reads=[t_qk[2 + pr][t // 4], t_c], writes=[tb[bi]], partial=pr > 0)
                            cp("act", ktok[:, t, :], bank_bf(bi)[:, 0:256], reads=[tb[bi]], writes=[t_ktok[t]])
                        zt = [al2("zt%d" % i, [128, 32], F32) for i in range(2)]
                        lf = [al2("lf%d" % i, [128, 16], F32) for i in range(2)]
                        eb = [al2("eb%d" % i, [128, 16], F32) for i in range(2)]
                        d1 = [al2("d1%d" % i, [128, 16], F32) for i in range(2)]
                        t_zt, t_lf, t_eb, t_d1 = (P.Ts(n, 2) for n in ("zt", "lf", "eb", "d1"))
                        for t in range(NT):
                            i = t % 2
                            xs_ = slice(t * 128, (t + 1) * 128)
                            bi = accring.next()
                            for k in range(16):
                                mm(banks[bi][:], xT[:, k, xs_], wsl[1][:, k, :], k == 0, k == 15, reads=[t_wsl[1], t_xT[t]],
                                   writes=[tb[bi]])
                            cp("act", vbase[:, t, :, 0:128], banks[bi][:].rearrange("p (a b) -> p a b", a=4), reads=[tb[bi]],
                               writes=[t_vb[t]], partial=True)
                            bi = accring.next()
                            for k in range(16):
                                mm(banks[bi][:], xT[:, k, xs_], wsl[2][:, k, :], k == 0, k == 15, reads=[t_wsl[2], t_xT[t]],
                                   writes=[tb[bi]])
                            act(so[:, t, :], banks[bi][:], AF.Sigmoid, reads=[tb[bi]], writes=[t_so[t]])
                            bi = accring.next()
                            for k in range(16):
                                mm(banks[bi][:, 0:32], xT[:, k, xs_], wgt[:, k, :], k == 0, k == 15, reads=[t_wgt, t_xT[t]],
                                   writes=[tb[bi]])
                            tt("dve", zt[i][:], banks[bi][:, 0:32], gbias[:], ALU.add, reads=[tb[bi], t_misc], writes=[t_zt[i]])
                            z4 = zt[i][:].rearrange("p (a b c) -> p a b c", a=2, b=2)
                            lf3 = lf[i][:].rearrange("p (a c) -> p a c", a=2)
                            act(lf3, z4[:, :, 1, :], AF.Exp, reads=[t_zt[i]], writes=[t_lf[i]], scale=-1.0)
                            act(lf[i][:], lf[i][:], AF.Ln, reads=[t_lf[i]], writes=[t_lf[i]], bias=1.0)
                            ts("dve", lf[i][:], lf[i][:], -1.0, ALU.mult, reads=[t_lf[i]], writes=[t_lf[i]])
                            bi = accring.next()
                            mm(banks[bi][:, 0:8], cf[:, MASKF, :], lf[i][:, 0:8], True, True, reads=[t_lf[i], t_c], writes=[tb[bi]])
                            mm(banks[bi][:, 8:16], cf[:, MASKB, :], lf[i][:, 8:16], True, True, reads=[t_lf[i], t_c],
                               writes=[tb[bi]], )
                            act(eb[i][:], banks[bi][:, 0:16], AF.Exp, reads=[tb[bi]], writes=[t_eb[i]])
                            ts("dve", gsm[:, t, 0, :], eb[i][:], 0.125, ALU.mult, reads=[t_eb[i]], writes=[t_gs[t]])
                            tt("dve", d1[i][:].rearrange("p (a c) -> p a c", a=2), z4[:, :, 0, :],
                               banks[bi][:, 0:16].rearrange("p (a c) -> p a c", a=2), ALU.subtract, reads=[t_zt[i], tb[bi]],
                               writes=[t_d1[i]])
                            act(gsm[:, t, 1, :], d1[i][:], AF.Exp, reads=[t_d1[i]], writes=[t_gs[t]], partial=True)
                            bi = accring.next()
                            mm(banks[bi][:, 0:8], cf[:, SELL, :], eb[i][:, 0:8], True, True, reads=[t_eb[i], t_c], writes=[tb[bi]])
                            mm(banks[bi][:, 8:16], cf[:, SELF, :], eb[i][:, 8:16], True, True, reads=[t_eb[i], t_c], writes=[tb[bi]])
                            cp("act", gsm[:, t, 3, :], banks[bi][:, 0:16], reads=[tb[bi]], writes=[t_gs[t]], partial=True)
                            tt("dve", gsm[:, t, 2, :], gsm[:, t, 1, :], banks[bi][:, 0:16], ALU.mult, reads=[tb[bi], t_gs[t]],
                               writes=[t_gs[t]], partial=True)
                        if dbg and seq == 0 and G == 0:
                            d_qk = dout("dbg_qk", [128, 4, S], BF16)
                            d_gs = dout("dbg_gs", [128, NT, 4, 16], F32)
                            d_kt = dout("dbg_ktok", [128, NT, 256], BF16)
                            dma("sp", d_qk, qkT[:], reads=[t for r in t_qk for t in r])
                            dma("sp", d_gs, gsm[:], reads=t_gs)
                            dma("sp", d_kt, ktok[:], reads=t_ktok)
                        P.flush()
                        es2.close()
                        hpart = al("hpart", [128, NT, 4, 128], BF16)
                        t_hp = [[P.T("hp%d_%d" % (t, h)) for h in range(4)] for t in range(NT)]
                        Cf = al("Cf", [128, 2, 2, 129], F32)
                        Cb = al("Cb", [128, 2, 2, 129], BF16)
                        t_C = [[[P.T("C%d%d%d" % (a, b, c)) for c in range(2)] for b in range(2)] for a in range(2)]
                        t_Cb = [[[P.T("Cb%d%d%d" % (a, b, c)) for c in range(2)] for b in range(2)] for a in range(2)]
                        P.op("dve", lambda e: e.memset(Cf[:], 0.0), writes=[t for a in t_C for b in a for t in b])
                        P.op("pool", lambda e: e.memset(Cb[:], 0.0), writes=[t for a in t_Cb for b in a for t in b])
                        NR = 4
                        sm = [al("sm%d" % i, [128, 128], BF16) for i in range(NR)]
                        v1 = [al("v1%d" % i, [128, 129], BF16) for i in range(NR)]
                        v2 = [al("v2%d" % i, [128, 129], BF16) for i in range(NR)]
                        dd = [al("dd%d" % i, [128, 2], F32) for i in range(NR)]
                        hs = [al("hs%d" % i, [128, 128], F32) for i in range(NR)]
                        hj = [al("hj%d" % i, [128, 128], F32) for i in range(NR)]
                        ssq = [al("ssq%d" % i, [128, 2], F32) for i in range(NR)]
                        t_sm, t_v1, t_v2, t_dd, t_hs, t_hj, t_ssq = (P.Ts(n, NR) for n in ("sm", "v1", "v2", "dd", "hs", "hj", "ssq"))
                        htok = [al("htok%d" % i, [128, 4, 128], BF16) for i in range(2)]
                        t_htok = P.Ts("htok", 2)
                        hTst = [al("hTst%d" % i, [128, 4, 128], BF16) for i in range(2)]
                        t_hTst = P.Ts("hTst", 2)
                        t_HT = P.T("HTb")
                        stq = Ring([0, 1])
                        oq = Ring([2, 3, 4])
                        uq = Ring([5, 6])
                        u = 0
                        done = [0] * NT
                        nfin = 0
                        for step in range(NT):
                            for dr in range(2):
                                c = step if dr == 0 else NT - 1 - step
                                cs = slice(c * 128, (c + 1) * 128)
                                for hl in range(4):
                                    pr, ho = hl // 2, 64 * (hl % 2)
                                    rows = slice(ho, ho + 64)
                                    col = dr * 8 + G * 4 + hl
                                    i = u % NR
                                    u += 1
                                    b_st = stq.next()
                                    mm(banks[b_st][:, 0:128], qkT[rows, 2 + pr, cs], qkT[rows, pr, cs], True, True,
                                       reads=[t_qk[2 + pr][c // 4], t_qk[pr][c // 4]], writes=[tb[b_st]])
                                    tt("dve", sm[i][:], banks[b_st][:, 0:128], cf[:, MASKF if dr == 0 else MASKB, :], ALU.mult,
                                       reads=[tb[b_st], t_c], writes=[t_sm[i]])
                                    ts("pool", v1[i][:], vbase[:, c, hl, :], gsm[:, c, 1, col:col + 1], ALU.mult,
                                       reads=[t_vb[c], t_gs[c]], writes=[t_v1[i]])
                                    ts("pool", v2[i][:], vbase[:, c, hl, :], gsm[:, c, 2, col:col + 1], ALU.mult,
                                       reads=[t_vb[c], t_gs[c]], writes=[t_v2[i]])
                                    b_o = oq.next()
                                    mm(banks[b_o][:, 0:129], sm[i][:], v1[i][:], True, False, reads=[t_sm[i], t_v1[i]],
                                       writes=[tb[b_o]])
                                    mm(banks[b_o][:, 0:129], qkT[rows, pr, cs], Cb[rows, pr, dr, :], False, True,
                                       reads=[t_qk[pr][c // 4], t_Cb[pr][dr][hl % 2]], writes=[tb[b_o]])
                                    b_u = uq.next()
                                    mm(banks[b_u][:, 0:129], ktok[:, c, pr * 128:(pr + 1) * 128], v2[i][:], True, True,
                                       reads=[t_ktok[c], t_v2[i]], writes=[tb[b_u]])
                                    tC = t_C[pr][dr][hl % 2]
                                    stt(Cf[rows, pr, dr, :], Cf[rows, pr, dr, :], gsm[rows, c, 3, col:col + 1], banks[b_u][rows, 0:129],
                                        ALU.mult, ALU.add, reads=[tC, t_gs[c], tb[b_u]], writes=[tC])
                                    cp("act", Cb[rows, pr, dr, :], Cf[rows, pr, dr, :], reads=[tC], writes=[t_Cb[pr][dr][hl % 2]])
                                    act(dd[i][:, 0:1], banks[b_o][:, 128:129], AF.Abs, reads=[tb[b_o], t_gs[c]], writes=[t_dd[i]],
                                        scale=gsm[:, c, 0, col:col + 1])
                                    ts("dve", dd[i][:, 0:1], dd[i][:, 0:1], 1.0, ALU.max, reads=[t_dd[i]], writes=[t_dd[i]])
                                    recip(dd[i][:, 0:1], dd[i][:, 0:1], reads=[t_dd[i]], writes=[t_dd[i]])
                                    ts("dve", dd[i][:, 1:2], dd[i][:, 0:1], gsm[:, c, 0, col:col + 1], ALU.mult,
                                       reads=[t_dd[i], t_gs[c]], writes=[t_dd[i]])
                                    if done[c] < 4:
                                        act(hpart[:, c, hl, :], banks[b_o][:, 0:128], AF.Copy, reads=[tb[b_o], t_dd[i]],
                                            writes=[t_hp[c][hl]], scale=dd[i][:, 1:2])
                                    else:
                                        stt(hs[i][:], banks[b_o][:, 0:128], dd[i][:, 1:2], hpart[:, c, hl, :], ALU.mult, ALU.add,
                                            reads=[tb[b_o], t_dd[i], t_hp[c][hl]], writes=[t_hs[i]])
                                        act(hj[i][:], hs[i][:], AF.Square, reads=[t_hs[i]], writes=[t_hj[i], t_ssq[i]],
                                            accum_out=ssq[i][:, 0:1])
                                        act(ssq[i][:, 1:2], ssq[i][:, 0:1], AF.Sqrt, reads=[t_ssq[i]], writes=[t_ssq[i]],
                                            bias=EPS, scale=1.0 / 128)
                                        recip(ssq[i][:, 1:2], ssq[i][:, 1:2], reads=[t_ssq[i]], writes=[t_ssq[i]])
                                        stt(hj[i][:], hs[i][:], ssq[i][:, 1:2], gml[:, hl * 128:(hl + 1) * 128], ALU.mult, ALU.mult,
                                            reads=[t_hs[i], t_ssq[i], t_misc], writes=[t_hj[i]])
                                        f2 = nfin % 2
                                        tt("pool", htok[f2][:, hl, :], hj[i][:], so[:, c, hl * 128:(hl + 1) * 128], ALU.mult,
                                           reads=[t_hj[i], t_so[c]], writes=[t_htok[f2]], partial=hl > 0)
                                        if hl == 3:
                                            b_t = uq.next()
                                            for j in range(4):
                                                P.op("pe", lambda e, b_t=b_t, j=j, f2=f2: e.transpose(
                                                    bank_bf(b_t)[:, j * 128:(j + 1) * 128], htok[f2][:, j, :], cb[:, IDENT, :]),
                                                    reads=[t_htok[f2], t_c], writes=[tb[b_t]], partial=j > 0)
                                            cp("act", hTst[f2][:], bank_bf(b_t)[:, 0:512].rearrange("p (a b) -> p a b", a=4),
                                               reads=[tb[b_t]], writes=[t_hTst[f2]])
                                            dma("sp", HT[seq, G * 512:(G + 1) * 512, cs].rearrange("(a p) t -> p a t", p=128),
                                                hTst[f2][:], reads=[t_hTst[f2]], writes=[t_HT], partial=True)
                                            nfin += 1
                                    done[c] += 1
                        P.flush()

        if "C" in stages:
            with ExitStack() as es:
                al = lambda name, shape, dt: es.enter_context(nc.sbuf_tensor(uq_name(name), shape, dt))
                wo = al("wo", [128, 16, D], BF16)
                wpg = al("wpg", [128, 16, D], BF16)
                wpp = al("wpp", [128, 2, D], BF16)
                wr = al("wr", [128, 16, NEXP], F32)
                lng = al("lng", [128, D], F32)
                lnb = al("lnb", [128, D], F32)
                bpgb = al("bpgb", [1, D], BF16)
                t_wo, t_wpg, t_wpp, t_wr, t_ln = P.T("wo"), P.T("wpg"), P.T("wpp"), P.T("wr"), P.T("ln1")
                for hf in range(4):
                    cs = slice(hf * 512, (hf + 1) * 512)
                    dma("pool", wo[:, :, cs], w_out[:, cs].rearrange("(c p) f -> p c f", p=128), writes=[t_wo], partial=hf > 0)
                for hf in range(4):
                    cs = slice(hf * 512, (hf + 1) * 512)
                    dma("pool", wpg[:, :, cs], wpg_d[:, cs].rearrange("(c p) f -> p c f", p=128), writes=[t_wpg], partial=hf > 0)
                dma("pool", wpp[:], wpp_d.rearrange("(c p) f -> p c f", p=128), writes=[t_wpp])
                if "bpgb" not in SKIP:
                    dma("pool", bpgb[:], bpg_d, writes=[t_wpp], partial=True)
                dma("sp", wr[:], wr_d.rearrange("(c p) f -> p c f", p=128), writes=[t_wr])
                dma("sp", lng[:], ln1g_d.partition_broadcast(128), writes=[t_ln])
                dma("sp", lnb[:], ln1b_d.partition_broadcast(128), writes=[t_ln], partial=True)
                hTt = al("hTt", [128, 16, 128], BF16)
                xt = al("xt", [128, D], F32)
                pt = al("pt", [128, 256], F32)
                x1b = al("x1b", [128, D], BF16)
                x1Tf = al("x1Tf", [128, 16, 128], F32)
                x1Tb = al("x1Tb", [128, 16, 128], BF16)
                pTb = al("pTb", [128, 2, 128], BF16)
                sg = al("sg", [128, 512], F32)
                plw = al("plw", [128, 512], F32)
                lnw = al("lnw", [128, 32], F32)
                rt = al("rt", [128, 24], F32)
                affp = al("affp", [128, 64], F32)
                t_hTt, t_xt, t_pt, t_x1b, t_x1Tf, t_x1Tb, t_pTb, t_sg, t_plw, t_lnw, t_rt, t_affp = (
                    P.T(n) for n in ("hTt", "xt", "pt", "x1b", "x1Tf", "x1Tb", "pTb", "sg", "plw", "lnw", "rt", "affp"))
                t_X1B, t_R = P.T("X1B"), P.T("Rw")
                P.op("dve", lambda e: e.memset(affp[:], 0.0), writes=[t_affp])
                bring = Ring(list(range(8)))
                for t in range(NT):
                    rows = slice(tok0 + t * 128, tok0 + (t + 1) * 128)
                    dma("sp", hTt[:], HT[seq].rearrange("(c p) t -> p c t", p=128)[:, :, t * 128:(t + 1) * 128], writes=[t_hTt])
                    dma("sp", xt[:], x_d[rows, :], writes=[t_xt])
                    dma("sp", pt[:], p_d[rows, :], writes=[t_pt])
                    for dg in range(4):
                        bi = bring.next()
                        cs = slice(dg * 512, (dg + 1) * 512)
                        for k in range(16):
                            mm(banks[bi][:], hTt[:, k, :], wo[:, k, cs], k == 0, k == 15, reads=[t_hTt, t_wo], writes=[tb[bi]])
                        stt(xt[:, cs], xt[:, cs], ALPHA, banks[bi][:], ALU.mult, ALU.add, reads=[t_xt, tb[bi]], writes=[t_xt],
                            partial=dg > 0)
                    if "ln" not in SKIP:
                        layernorm(xt, t_xt, lng, lnb, t_ln, lnw, t_lnw)
                    if "x1b" not in SKIP:
                        cp("act", x1b[:], xt[:], reads=[t_xt], writes=[t_x1b])
                        dma("sp", X1B[rows, :], x1b[:], reads=[t_x1b], writes=[t_X1B], partial=True)
                    for g in range(4 if "xtr" not in SKIP else 0):
                        bi = bring.next()
                        for j in range(4):
                            c = g * 4 + j
                            P.op("pe", lambda e, bi=bi, j=j, c=c: e.transpose(
                                banks[bi][:, j * 128:(j + 1) * 128], xt[:, c * 128:(c + 1) * 128], cf[:, IDENT, :]),
                                reads=[t_xt, t_c], writes=[tb[bi]], partial=j > 0)
                        pv = banks[bi][:].rearrange("p (a b) -> p a b", a=4)
                        cp("act", x1Tf[:, g * 4:(g + 1) * 4, :], pv, reads=[tb[bi]], writes=[t_x1Tf], partial=g > 0)
                        if "x1tb" not in SKIP:
                            cp("dve", x1Tb[:, g * 4:(g + 1) * 4, :], x1Tf[:, g * 4:(g + 1) * 4, :], reads=[t_x1Tf], writes=[t_x1Tb],
                               partial=g > 0)
                    if "router" not in SKIP:
                        bi = bring.next()
                        for k in range(16):
                            mm(banks[bi][:, 0:NEXP], x1Tf[:, k, :], wr[:, k, :], k == 0, k == 15, reads=[t_x1Tf, t_wr], writes=[tb[bi]])
                        P.op("dve", lambda e, bi=bi: e.tensor_reduce(out=rt[:, 16:17], in_=banks[bi][:, 0:NEXP],
                                                                      axis=mybir.AxisListType.X, op=ALU.max, negate=True),
                             reads=[tb[bi]], writes=[t_rt])
                        act(rt[:, 0:NEXP], banks[bi][:, 0:NEXP], AF.Exp, reads=[tb[bi], t_rt], writes=[t_rt], bias=rt[:, 16:17],
                            accum_out=rt[:, 17:18])
                        recip(rt[:, 18:19], rt[:, 17:18], reads=[t_rt], writes=[t_rt])
                        ts("dve", affp[:, 32 * seq:32 * seq + NEXP], rt[:, 0:NEXP], rt[:, 18:19], ALU.mult, reads=[t_rt],
                           writes=[t_affp])
                        bi = bring.next()
                        mm(banks[bi][0:64, 0:128], affp[:], cf[:, IDENT, :], True, True, reads=[t_affp, t_c], writes=[tb[bi]])
                        cp("act", affT[32 * seq:32 * seq + NEXP, t * 128:(t + 1) * 128], banks[bi][32 * seq:32 * seq + NEXP, 0:128],
                           reads=[tb[bi]], writes=[t_affT], partial=True)
                    if "pl" not in SKIP:
                        bi = bring.next()
                        for j in range(2):
                            P.op("pe", lambda e, bi=bi, j=j: e.transpose(
                                banks[bi][:, j * 128:(j + 1) * 128], pt[:, j * 128:(j + 1) * 128], cf[:, IDENT, :]),
                                reads=[t_pt, t_c], writes=[tb[bi]], partial=j > 0)
                        cp("act", pTb[:], banks[bi][:, 0:256].rearrange("p (a b) -> p a b", a=2), reads=[tb[bi]], writes=[t_pTb])
                        for dg in range(4):
                            cs = slice(dg * 512, (dg + 1) * 512)
                            bi = bring.next()
                            for k in range(16):
                                mm(banks[bi][:], x1Tb[:, k, :], wpg[:, k, cs], k == 0, False, reads=[t_x1Tb, t_wpg], writes=[tb[bi]])
                            mm(banks[bi][:], onesb[0:1, :], bpgb[0:1, cs], False, True, reads=[t_c, t_wpp], writes=[tb[bi]])
                            act(sg[:], banks[bi][:], AF.Sigmoid, reads=[tb[bi]], writes=[t_sg])
                            b2 = bring.next()
                            for k in range(2):
                                mm(banks[b2][:], pTb[:, k, :], wpp[:, k, cs], k == 0, k == 1, reads=[t_pTb, t_wpp], writes=[tb[b2]])
                            tt("dve", plw[:], sg[:], banks[b2][:], ALU.mult, reads=[t_sg, tb[b2]], writes=[t_plw])
                            stt(xt[:, cs], xt[:, cs], ALPHA, plw[:], ALU.mult, ALU.add, reads=[t_xt, t_plw], writes=[t_xt])
                    dma("sp", R[rows, :], xt[:], reads=[t_xt], writes=[t_R], partial=True)
                P.flush()

    if "M" in stages:
        with ExitStack() as es:
            al = lambda name, shape, dt: es.enter_context(nc.sbuf_tensor(uq_name(name), shape, dt))
            NC_ = 2 * nseq
            NCOL = 128 * NC_
            tkw = al("tkw", [64, S], F32)
            top = al("top", [64, CAP], F32)
            idxu = al("idxu", [64, CAP], U32)
            idxf = al("idxf", [64, CAP], F32)
            idxT = al("idxT", [128, 2, 64], I32)
            gateT = al("gateT", [128, 2, 64], F32)
            t_tk, t_top, t_idxu, t_idxf, t_idxT, t_gateT = (P.T(n) for n in ("tkw", "top", "idxu", "idxf", "idxT", "gateT"))
            cp("dve", tkw[:], affT[:], reads=[t_affT], writes=[t_tk])
            for r in range(CAP // 8):
                rs = slice(r * 8, (r + 1) * 8)
                P.op("dve", lambda e, rs=rs: e.max(out=top[:, rs], in_=tkw[:]), reads=[t_tk], writes=[t_top], partial=True)
                P.op("dve", lambda e, rs=rs: e.max_index(out=idxu[:, rs], in_max=top[:, rs], in_values=tkw[:]),
                     reads=[t_tk, t_top], writes=[t_idxu], partial=True)
                P.op("dve", lambda e, rs=rs: e.match_replace(out=tkw[:], in_to_replace=top[:, rs], in_values=tkw[:], imm_value=-1.0),
                     reads=[t_top, t_tk], writes=[t_tk])
            cp("dve", idxf[:], idxu[:], reads=[t_idxu], writes=[t_idxf])
            if nseq > 1:
                ts("dve", idxf[32:48, :], idxf[32:48, :], float(S), ALU.add, reads=[t_idxf], writes=[t_idxf])
            for ct in range(2):
                P.op("pe", lambda e, ct=ct: e.transpose(banks[0][:, ct * 64:(ct + 1) * 64], idxf[:, ct * 128:(ct + 1) * 128],
                                                      cf[0:64, IDENT, 0:64]), reads=[t_idxf, t_c], writes=[tb[0]], partial=ct > 0)
                P.op("pe", lambda e, ct=ct: e.transpose(banks[1][:, ct * 64:(ct + 1) * 64], top[:, ct * 128:(ct + 1) * 128],
                                                      cf[0:64, IDENT, 0:64]), reads=[t_top, t_c], writes=[tb[1]], partial=ct > 0)
            cp("dve", idxT[:], banks[0][:, 0:128].rearrange("p (a b) -> p a b", a=2), reads=[tb[0]], writes=[t_idxT])
            cp("act", gateT[:], banks[1][:, 0:128].rearrange("p (a b) -> p a b", a=2), reads=[tb[1]], writes=[t_gateT])
            if dbg:
                d_idx = dout("dbg_idxT", [128, 2, 64], I32)
                d_gate = dout("dbg_gateT", [128, 2, 64], F32)
                dma("sp", d_idx, idxT[:], reads=[t_idxT])
                dma("sp", d_gate, gateT[:], reads=[t_gateT])
            NSL = 7
            wsl = [al("wm%d" % i, [128, 8192], BF16) for i in range(NSL)]
            t_wsl = P.Ts("wm", NSL)
            xs = [al("xs%d" % i, [128, D], BF16) for i in range(NC_)]
            t_xs = P.Ts("xs", NC_)
            xsT = al("xsT", [128, 16, NCOL], BF16)
            t_xsT = P.Ts("xsT", NC_)
            hT = al("hT", [128, 8, NCOL], BF16)
            t_hT = P.Ts("hT", 8)
            sgw = [al("sgw%d" % i, [128, NCOL], F32) for i in range(2)]
            t_sgw = P.Ts("sgw", 2)
            yst = [al("yst%d" % i, [128, D], F32) for i in range(2)]
            t_yst = P.Ts("yst", 2)
            t_Rm = P.T("Rm")
            t_X1Bm = P.T("X1Bm")

            def piece_ap(n):
                e, i = divmod(n, 6)
                if i < 4:
                    src = (wg_d if i % 2 == 0 else wu_d)[e][:, (i // 2) * 512:(i // 2 + 1) * 512]
                    return src.rearrange("(c p) f -> p c f", p=128), "p (k f) -> p k f", 16
                hh = i - 4
                return wd_d[e][:, hh * 1024:(hh + 1) * 1024].rearrange("(c p) f -> p c f", p=128), "p (k f) -> p k f", 8

            def load_piece(n):
                if n >= 6 * NEXP:
                    return
                src, pat, kk = piece_ap(n)
                sl = n % NSL
                dma("pool", wsl[sl][:].rearrange(pat, k=kk), src, writes=[t_wsl[sl]])

            def wview(n, kk):
                return wsl[n % NSL][:].rearrange("p (k f) -> p k f", k=kk), t_wsl[n % NSL]

            for n in range(NSL):
                load_piece(n)
            bring = Ring(list(range(8)))
            sgi = 0
            for e in range(NEXP):
                cols = []
                for j in range(NC_):
                    sq_, ct = divmod(j, 2)
                    col = 32 * sq_ + e
                    cols.append((ct, col))
                    P.op("pool", lambda eng, j=j, ct=ct, col=col: eng.indirect_dma_start(
                        out=xs[j][:], out_offset=None, in_=X1B,
                        in_offset=bass.IndirectOffsetOnAxis(ap=idxT[:, ct, col:col + 1], axis=0)),
                        reads=[t_idxT, t_X1Bm], writes=[t_xs[j]], dma=True)
                for j in range(NC_):
                    for g4 in range(4):
                        bi = bring.next()
                        for q4 in range(4):
                            c = g4 * 4 + q4
                            P.op("pe", lambda eng, bi=bi, q4=q4, c=c, j=j: eng.transpose(
                                bank_bf(bi)[:, q4 * 128:(q4 + 1) * 128], xs[j][:, c * 128:(c + 1) * 128], cb[:, IDENT, :]),
                                reads=[t_xs[j], t_c], writes=[tb[bi]], partial=q4 > 0)
                        cp("act" if g4 % 2 else "dve", xsT[:, g4 * 4:(g4 + 1) * 4, j * 128:(j + 1) * 128],
                           bank_bf(bi)[:, 0:512].rearrange("p (a b) -> p a b", a=4), reads=[tb[bi]], writes=[t_xsT[j]],
                           partial=g4 > 0)
                for fh in range(2):
                    ng, nu = 6 * e + 2 * fh, 6 * e + 2 * fh + 1
                    wgv, twg = wview(ng, 16)
                    wuv, twu = wview(nu, 16)
                    for ft in range(4):
                        f = fh * 4 + ft
                        bg = bring.next()
                        for k in range(16):
                            mm(banks[bg][:, 0:NCOL], wgv[:, k, ft * 128:(ft + 1) * 128], xsT[:, k, :], k == 0, k == 15,
                               reads=[twg] + t_xsT, writes=[tb[bg]])
                        bu = bring.next()
                        for k in range(16):
                            mm(banks[bu][:, 0:NCOL], wuv[:, k, ft * 128:(ft + 1) * 128], xsT[:, k, :], k == 0, k == 15,
                               reads=[twu] + t_xsT, writes=[tb[bu]])
                        si = sgi % 2
                        sgi += 1
                        act(sgw[si][:], banks[bg][:, 0:NCOL], AF.Silu, reads=[tb[bg]], writes=[t_sgw[si]])
                        tt("dve", hT[:, f, :], sgw[si][:], banks[bu][:, 0:NCOL], ALU.mult, reads=[t_sgw[si], tb[bu]],
                           writes=[t_hT[f]])
                    load_piece(ng + NSL)
                    load_piece(nu + NSL)
                nd0, nd1 = 6 * e + 4, 6 * e + 5
                for j in range(NC_):
                    ct, col = cols[j]
                    yi = (e * NC_ + j) % 2
                    for dh, nd in enumerate((nd0, nd1)):
                        wdv, twd = wview(nd, 8)
                        for dg2 in range(2):
                            bi = bring.next()
                            for k in range(8):
                                mm(banks[bi][:], hT[:, k, j * 128:(j + 1) * 128], wdv[:, k, dg2 * 512:(dg2 + 1) * 512], k == 0, k == 7,
                                   reads=[t_hT[k], twd], writes=[tb[bi]])
                            oc = dh * 1024 + dg2 * 512
                            act(yst[yi][:, oc:oc + 512], banks[bi][:], AF.Copy, reads=[tb[bi], t_gateT], writes=[t_yst[yi]],
                                scale=gateT[:, ct, col:col + 1], partial=(dh + dg2) > 0)
                    P.op("pool", lambda eng, yi=yi, ct=ct, col=col: eng.indirect_dma_start(
                        out=R, out_offset=bass.IndirectOffsetOnAxis(ap=idxT[:, ct, col:col + 1], axis=0),
                        in_=yst[yi][:], in_offset=None, compute_op=ALU.add),
                        reads=[t_idxT, t_yst[yi]], writes=[t_Rm], dma=True)
                load_piece(nd0 + NSL)
                load_piece(nd1 + NSL)
            P.flush()

    if "F" in stages:
        with ExitStack() as es:
            al = lambda name, shape, dt: es.enter_context(nc.sbuf_tensor(uq_name(name), shape, dt))
            lng = al("ln2g", [128, D], F32)
            lnb = al("ln2b", [128, D], F32)
            t_ln = P.T("ln2")
            dma("sp", lng[:], ln2g_d.partition_broadcast(128), writes=[t_ln])
            dma("sp", lnb[:], ln2b_d.partition_broadcast(128), writes=[t_ln], partial=True)
            NB = 4
            rt_ = [al("rf%d" % i, [128, D], F32) for i in range(NB)]
            lw = [al("lw%d" % i, [128, 32], F32) for i in range(NB)]
            t_rt_ = P.Ts("rf", NB)
            t_lw = P.Ts("lw", NB)
            t_out = P.T("out")
            for t in range(nseq * NT):
                i = t % NB
                rows = slice(t * 128, (t + 1) * 128)
                dma("sp", rt_[i][:], R[rows, :], writes=[t_rt_[i]])
                layernorm(rt_[i], t_rt_[i], lng, lnb, t_ln, lw[i], t_lw[i])
                dma("sp", out_d[rows, :], rt_[i][:], reads=[t_rt_[i]], writes=[t_out], partial=True)
    P.flush(final=True)
    return nc, dbg_out, P


def _consts():
    c = np.zeros((6, 128, 128), np.float32)
    i = np.arange(128)
    c[0] = np.eye(128, dtype=np.float32)
    c[1] = (i[:, None] <= i[None, :]).astype(np.float32)
    c[2] = (i[:, None] >= i[None, :]).astype(np.float32)
    c[3][127, :] = 1.0
    c[4][0, :] = 1.0
    partner = np.where((i % 64) < 32, i + 32, i - 32)
    c[5][partner, i] = 1.0
    t = np.arange(S)
    row = (t // 64).astype(np.float32)
    colp = (t % 64).astype(np.float32)
    inv = (np.float32(10000.0) ** (-np.arange(32, dtype=np.float32) / np.float32(32))).astype(np.float32)
    cosT = np.zeros((128, S), np.float32)
    sinT = np.zeros((128, S), np.float32)
    for d in range(128):
        pos = row if d < 64 else colp
        ang = (pos * inv[d % 32]).astype(np.float32)
        cosT[d] = np.cos(ang)
        sgn = -1.0 if (d % 64) < 32 else 1.0
        sinT[d] = sgn * np.sin(ang)
    return c, cosT, sinT


def prep_shared(inp):
    f = lambda a: np.ascontiguousarray(np.asarray(a, dtype=np.float32))
    c, cosT, sinT = _consts()
    bi = f(inp["b_igate"])[0]
    bf = f(inp["b_fgate"])[0]
    gb = np.stack([bi, bf], axis=1).reshape(1, 32)
    return {
        "w_in": f(inp["w_in"])[0], "convT": f(np.asarray(inp["conv_w"])[0].T), "gbias": f(gb),
        "g_mlstm": f(inp["g_mlstm"]).reshape(1, 1024), "g_q": f(inp["g_q"]).reshape(128, 1),
        "g_k": f(inp["g_k"]).reshape(128, 1), "w_out": f(inp["w_out"])[0],
        "ln1_g": f(inp["ln1_g"]).reshape(1, D), "ln1_b": f(inp["ln1_b"]).reshape(1, D),
        "w_router": f(inp["w_router"])[0], "w_gate": f(inp["w_gate"])[0], "w_up": f(inp["w_up"])[0],
        "w_down": f(inp["w_down"])[0], "w_pl_proj": f(inp["w_pl_proj"])[0], "w_pl_gate": f(inp["w_pl_gate"])[0],
        "b_pl_gate": f(inp["b_pl_gate"]).reshape(1, D), "ln2_g": f(inp["ln2_g"]).reshape(1, D),
        "ln2_b": f(inp["ln2_b"]).reshape(1, D), "cst": c, "cosT": cosT, "sinT": sinT,
    }


_CACHE = {}


def kernel(**inputs):
    ncores = 8
    sh = prep_shared(inputs)
    x = np.asarray(inputs["x"], dtype=np.float32)
    p = np.asarray(inputs["p"], dtype=np.float32)[0]
    nseq = x.shape[0] // ncores
    if "nc" not in _CACHE:
        _CACHE["nc"] = build(nseq=nseq, stages="ABCMF", dbg=False)[0]
    nc = _CACHE["nc"]
    in_maps = []
    for c in range(ncores):
        m = dict(sh)
        m["x"] = np.ascontiguousarray(x[c * nseq:(c + 1) * nseq].reshape(nseq * S, D))
        m["p"] = np.ascontiguousarray(p[c * nseq:(c + 1) * nseq].reshape(nseq * S, 256))
        in_maps.append(m)
    res = run_bass_kernel_spmd(nc, in_maps, core_ids=list(range(ncores)))
    out = np.concatenate([np.asarray(r["out"]).reshape(nseq, S, D) for r in res.results], axis=0)
    return out.astype(np.float32)
```

```python
import math
from contextlib import ExitStack
import numpy as np
import concourse.bass as bass
import concourse.mybir as mybir
from concourse.alu_op_type import AluOpType as ALU
from concourse.bass_utils import run_bass_kernel_spmd

AF = mybir.ActivationFunctionType
F32 = mybir.dt.float32
BF16 = mybir.dt.bfloat16
I32 = mybir.dt.int32
U32 = mybir.dt.uint32

ENGS = ("pe", "act", "dve", "pool", "sp")
D = 2048
S = 2048
NT = 16
NEXP = 16
CAP = 256
FF = 1024
EPS = 1e-6
ALPHA = 2.0 ** 0.25


class T:
    __slots__ = ("name", "w", "r")

    def __init__(self, name):
        self.name = name
        self.w = []
        self.r = []


class Op:
    __slots__ = ("idx", "eng", "fn", "deps", "dma", "flag", "evt")

    def __init__(self, idx, eng, fn, deps, dma):
        self.idx = idx
        self.eng = eng
        self.fn = fn
        self.deps = deps
        self.dma = dma
        self.flag = False
        self.evt = None


class Prog:
    def __init__(self, nc, n_dma_sems=32):
        self.nc = nc
        self.ops = []
        self.q = {e: [] for e in ENGS}
        self.tiles = []
        self.nd = n_dma_sems
        self.esem = {e: nc.alloc_semaphore("s_" + e) for e in ENGS}
        self.dsems = [nc.alloc_semaphore("d%d" % i) for i in range(n_dma_sems)]
        self.dval = [0] * n_dma_sems
        self.dnext = {"sp": 0, "pool": 0}
        self.cnt = {e: 0 for e in ENGS}
        self.di = 0
        self.known = {e: {} for e in ENGS}
        self.nins = 0

    def T(self, name):
        t = T(name)
        self.tiles.append(t)
        return t

    def Ts(self, name, n):
        return [self.T("%s%d" % (name, i)) for i in range(n)]

    def op(self, eng, fn, reads=(), writes=(), dma=False, partial=False):
        idx = len(self.ops)
        deps = set()
        for t in reads:
            deps.update(t.w)
        for t in writes:
            deps.update(t.w)
            deps.update(t.r)
        o = Op(idx, eng, fn, deps, dma)
        for t in reads:
            t.r.append(idx)
        for t in writes:
            if partial:
                t.w.append(idx)
            else:
                t.w = [idx]
            t.r = []
        self.ops.append(o)
        self.q[eng].append(o)
        return o

    def flush(self, final=False):
        nc = self.nc
        ops = self.ops
        alld = set(o.idx for o in ops if o.dma)
        for e in ENGS:
            if self.q[e]:
                alld.add(self.q[e][-1].idx)
        for e in ENGS:
            o = Op(len(ops), e, lambda eng: eng.nop(), set(alld), False)
            ops.append(o)
            self.q[e].append(o)
        for o in ops:
            best = {}
            keep = set()
            for d in o.deps:
                p = ops[d]
                if p.dma:
                    keep.add(d)
                    continue
                if p.eng == "pe" and o.eng == "pe" and not o.dma:
                    continue
                b = best.get(p.eng)
                if b is None or d > b:
                    best[p.eng] = d
            keep.update(best.values())
            o.deps = keep
            for d in keep:
                ops[d].flag = True
        dma_prev = {}
        for o in ops:
            if o.dma:
                half = self.nd // 2
                s = (self.dnext[o.eng] % half) + (0 if o.eng == "sp" else half)
                self.dnext[o.eng] += 1
                dma_prev[o.idx] = (self.dsems[s], self.dval[s])
                self.dval[s] += 16
                o.evt = (self.dsems[s], self.dval[s])
            elif o.flag:
                self.cnt[o.eng] += 1
                o.evt = (self.esem[o.eng], self.cnt[o.eng])
        finals = [(self.dsems[i], self.dval[i]) for i in range(self.nd) if self.dval[i]]

        def run_queue(ename, eng):
            known = self.known[ename]
            for o in self.q[ename]:
                need = {}
                ws = []
                if o.dma:
                    ws.append(dma_prev[o.idx])
                for d in o.deps:
                    ws.append(ops[d].evt)
                for (s, v) in ws:
                    if v <= 0:
                        continue
                    k = id(s)
                    if known.get(k, 0) >= v:
                        continue
                    if k not in need or need[k][1] < v:
                        need[k] = (s, v)
                for k, (s, v) in need.items():
                    known[k] = v
                    eng.wait_ge(s, v)
                    self.nins += 1
                ins = o.fn(eng)
                self.nins += 1
                if o.dma:
                    ins.then_inc(o.evt[0], 16)
                elif o.flag:
                    ins.then_inc(o.evt[0], 1)
            if final and ename == "sp":
                for (s, v) in finals:
                    eng.wait_ge(s, v)

        with nc.Block() as block:
            @block.tensor
            def _(e):
                run_queue("pe", e)

            @block.scalar
            def _(e):
                run_queue("act", e)

            @block.vector
            def _(e):
                run_queue("dve", e)

            @block.gpsimd
            def _(e):
                run_queue("pool", e)

            @block.sync
            def _(e):
                run_queue("sp", e)
        self.ops = []
        self.q = {e: [] for e in ENGS}
        for t in self.tiles:
            t.w = []
            t.r = []


class Ring:
    def __init__(self, items):
        self.items = items
        self.i = 0

    def next(self):
        it = self.items[self.i % len(self.items)]
        self.i += 1
        return it


def build(nseq=2, stages="ABCMF", dbg=False):
    import os
    SKIP = set(os.environ.get("KSKIP", "").split(","))
    nc = bass.Bass("TRN2", target_bir_lowering=False)
    NTOK = nseq * S
    P = Prog(nc)

    def din(name, shape, dt=F32):
        return nc.dram_tensor(name, list(shape), dt, kind="ExternalInput").ap()

    x_d = din("x", [NTOK, D])
    p_d = din("p", [NTOK, 256])
    w_in = din("w_in", [D, 4640])
    convT = din("convT", [1024, 5])
    gbias_d = din("gbias", [1, 32])
    gml_d = din("g_mlstm", [1, 1024])
    gq_d = din("g_q", [128, 1])
    gk_d = din("g_k", [128, 1])
    w_out = din("w_out", [D, D])
    ln1g_d = din("ln1_g", [1, D])
    ln1b_d = din("ln1_b", [1, D])
    wr_d = din("w_router", [D, NEXP])
    wg_d = din("w_gate", [NEXP, D, FF])
    wu_d = din("w_up", [NEXP, D, FF])
    wd_d = din("w_down", [NEXP, FF, D])
    wpp_d = din("w_pl_proj", [256, D])
    wpg_d = din("w_pl_gate", [D, D])
    bpg_d = din("b_pl_gate", [1, D])
    ln2g_d = din("ln2_g", [1, D])
    ln2b_d = din("ln2_b", [1, D])
    cst_d = din("cst", [6, 128, 128])
    cos_d = din("cosT", [128, S])
    sin_d = din("sinT", [128, S])
    out_d = nc.dram_tensor("out", [NTOK, D], F32, kind="ExternalOutput").ap()
    ikind = "ExternalOutput" if dbg else "Internal"
    HT = nc.dram_tensor("HT", [nseq, D, S], BF16, kind=ikind).ap()
    X1B = nc.dram_tensor("X1B", [NTOK, D], BF16, kind=ikind).ap()
    R = nc.dram_tensor("R", [NTOK, D], F32, kind=ikind).ap()
    dbg_out = {}
    _uq = [0]

    def uq_name(n):
        _uq[0] += 1
        return "%s_u%d" % (n, _uq[0])

    def dout(name, shape, dt=F32):
        a = nc.dram_tensor(name, list(shape), dt, kind="ExternalOutput").ap()
        dbg_out[name] = a
        return a

    cf = nc.alloc_sbuf_tensor("cf", [128, 6, 128], F32)
    cb = nc.alloc_sbuf_tensor("cb", [128, 6, 128], BF16)
    onesb = nc.alloc_sbuf_tensor("onesb", [128, 128], BF16)
    t_c = P.T("consts")
    IDENT, MASKF, MASKB, SELL, SELF, PERM = range(6)
    P.op("sp", lambda e: e.dma_start(out=cf[:], in_=cst_d.rearrange("c p f -> p c f")), writes=[t_c], dma=True)
    P.op("dve", lambda e: e.tensor_copy(out=cb[:], in_=cf[:]), reads=[t_c], writes=[t_c], partial=True)
    P.op("dve", lambda e: e.memset(onesb[:], 1.0), writes=[t_c], partial=True)

    banks = [nc.alloc_psum_tensor("bank%d" % i, [128, 512], F32) for i in range(8)]
    tb = P.Ts("bank", 8)

    def bank_bf(i):
        return banks[i][:].bitcast(BF16)

    def dma(q, out, in_, reads=(), writes=(), partial=False):
        return P.op(q, lambda e: e.dma_start(out=out, in_=in_), reads=reads, writes=writes, dma=True, partial=partial)

    def mm(out, lhsT, rhs, start, stop, reads, writes):
        return P.op("pe", lambda e: e.matmul(out, lhsT, rhs, start=start, stop=stop), reads=reads, writes=writes,
                    partial=not start)

    def act(out, in_, func, reads, writes, bias=None, scale=None, accum_out=None, partial=False):
        kw = {}
        if bias is not None:
            kw["bias"] = bias
        if scale is not None:
            kw["scale"] = scale
        if accum_out is not None:
            kw["accum_out"] = accum_out
        return P.op("act", lambda e: e.activation(out, in_, func, **kw), reads=reads, writes=writes, partial=partial)

    def ts(eng, out, in0, s1, op0, reads, writes, s2=None, op1=None, partial=False):
        if op1 is None:
            return P.op(eng, lambda e: e.tensor_scalar(out, in0, s1, None, op0), reads=reads, writes=writes, partial=partial)
        return P.op(eng, lambda e: e.tensor_scalar(out, in0, s1, s2, op0, op1), reads=reads, writes=writes, partial=partial)

    def tt(eng, out, in0, in1, op, reads, writes, partial=False):
        return P.op(eng, lambda e: e.tensor_tensor(out, in0, in1, op), reads=reads, writes=writes, partial=partial)

    def stt(out, in0, scalar, in1, op0, op1, reads, writes, partial=False):
        return P.op("dve", lambda e: e.scalar_tensor_tensor(out, in0, scalar, in1, op0, op1), reads=reads, writes=writes,
                    partial=partial)

    def cp(eng, out, in_, reads, writes, partial=False):
        if eng == "act":
            return P.op("act", lambda e: e.copy(out, in_), reads=reads, writes=writes, partial=partial)
        return P.op(eng, lambda e: e.tensor_copy(out=out, in_=in_), reads=reads, writes=writes, partial=partial)

    def recip(out, in_, reads, writes):
        return P.op("dve", lambda e: e.reciprocal(out, in_), reads=reads, writes=writes)

    def load_w(slot, tslot, pieces):
        first = True
        for (c0, ap) in pieces:
            n = ap.shape[1]
            kc = ap.shape[0] // 128
            dma("pool", slot[:, 0:kc, c0:c0 + n], ap.rearrange("(c p) f -> p c f", p=128), writes=[tslot],
                partial=not first)
            first = False

    def bcast_row(dst, row_ap, tdst):
        dma("sp", dst, row_ap.partition_broadcast(128), writes=[tdst])

    affT = nc.alloc_sbuf_tensor("affT", [64, S], F32)
    t_affT = P.T("affT")
    P.op("pool", lambda e: e.memset(affT[:], 0.0), writes=[t_affT])

    def layernorm(x, t_x, g, b, t_gb, w, t_w):
        st = w[:, 0:24].rearrange("p (a b) -> p a b", a=4)
        for c in range(4):
            P.op("dve", lambda e, c=c: e.bn_stats(out=st[:, c, :], in_=x[:, c * 512:(c + 1) * 512]), reads=[t_x], writes=[t_w],
                 partial=c > 0)
        P.op("dve", lambda e: e.bn_aggr(out=w[:, 24:26], in_=w[:, 0:24]), reads=[t_w], writes=[t_w])
        act(w[:, 26:27], w[:, 25:26], AF.Sqrt, reads=[t_w], writes=[t_w], bias=EPS, scale=1.0)
        recip(w[:, 26:27], w[:, 26:27], reads=[t_w], writes=[t_w])
        ts("dve", x[:], x[:], w[:, 24:25], ALU.subtract, reads=[t_x, t_w], writes=[t_x], s2=w[:, 26:27], op1=ALU.mult)
        tt("dve", x[:], x[:], g[:], ALU.mult, reads=[t_x, t_gb], writes=[t_x])
        tt("dve", x[:], x[:], b[:], ALU.add, reads=[t_x, t_gb], writes=[t_x])

    P.flush()

    for seq in range(nseq):
        tok0 = seq * S
        with ExitStack() as seq_es:
            if any(c in stages for c in "AB"):
                xT = seq_es.enter_context(nc.sbuf_tensor(uq_name("xT"), [128, 16, S], BF16))
                t_xT = P.Ts("xT", NT)
                with ExitStack() as es:
                    xin = [es.enter_context(nc.sbuf_tensor(uq_name("xin%d" % i), [128, D], F32)) for i in range(2)]
                    t_xin = P.Ts("xin", 2)
                    k = 0
                    for t in range(NT):
                        b = t % 2
                        dma("sp", xin[b][:], x_d[tok0 + t * 128: tok0 + (t + 1) * 128, :], writes=[t_xin[b]])
                        for g in range(4):
                            bi = k % 4
                            k += 1
                            for j in range(4):
                                c = g * 4 + j
                                P.op("pe", lambda e, bi=bi, j=j, b=b, c=c: e.transpose(
                                    banks[bi][:, j * 128:(j + 1) * 128], xin[b][:, c * 128:(c + 1) * 128], cf[:, IDENT, :]),
                                    reads=[t_xin[b], t_c], writes=[tb[bi]], partial=j > 0)
                            cp("act" if g % 2 == 0 else "dve", xT[:, g * 4:(g + 1) * 4, t * 128:(t + 1) * 128],
                               banks[bi][:].rearrange("p (a b) -> p a b", a=4), reads=[tb[bi]], writes=[t_xT[t]],
                               partial=g > 0)
                    P.flush()

            if "A" in stages:
                with ExitStack() as es:
                    al = lambda name, shape, dt: es.enter_context(nc.sbuf_tensor(uq_name(name), shape, dt))
                    qT = al("qT", [128, 8, S], BF16)
                    kT = al("kT", [128, 2, S], BF16)
                    vtok = al("vtok", [128, NT, 256], BF16)
                    cosS = al("cosS", [128, S], F32)
                    sinS = al("sinS", [128, S], F32)
                    gqk = al("gqk", [128, 2], F32)
                    es2 = ExitStack()
                    al2 = lambda name, shape, dt: es2.enter_context(nc.sbuf_tensor(uq_name(name), shape, dt))
                    wsl = [al2("wa%d" % i, [128, 16, 512], BF16) for i in range(3)]
                    t_wsl = P.Ts("wa", 3)
                    wring = Ring(list(zip(wsl, t_wsl)))
                    t_qT = [[P.T("qT%d_%d" % (h, b)) for b in range(4)] for h in range(8)]
                    t_kT = [[P.T("kT%d_%d" % (h, b)) for b in range(4)] for h in range(2)]
                    t_v = P.Ts("vtok", NT)
                    t_tab = P.T("tab")
                    dma("sp", cosS[:], cos_d, writes=[t_tab])
                    dma("sp", sinS[:], sin_d, writes=[t_tab], partial=True)
                    dma("sp", gqk[:, 0:1], gq_d, writes=[t_tab], partial=True)
                    dma("sp", gqk[:, 1:2], gk_d, writes=[t_tab], partial=True)
                    ts("dve", gqk[:, 0:1], gqk[:, 0:1], 128.0 ** -0.5, ALU.mult, reads=[t_tab], writes=[t_tab], partial=True)
                    nw = 2
                    qf = [al2("qf%d" % i, [128, 512], F32) for i in range(nw)]
                    sq = [al2("sq%d" % i, [128, 512], BF16) for i in range(nw)]
                    sd = [al2("sd%d" % i, [128, 512], F32) for i in range(nw)]
                    qn = [al2("qn%d" % i, [128, 512], BF16) for i in range(nw)]
                    t1 = [al2("t1%d" % i, [128, 512], F32) for i in range(1)] * nw
                    t2 = [al2("t2%d" % i, [128, 512], F32) for i in range(1)] * nw
                    t_qf, t_sq, t_sd, t_qn = (P.Ts(n, nw) for n in ("qf", "sq", "sd", "qn"))
                    t_t1 = P.Ts("t1", 1) * nw
                    t_t2 = P.Ts("t2", 1) * nw
                    nrc = [0]
                    accring = Ring([0, 1])
                    auxring = Ring([2, 3])

                    def normrope(bi, out_ap, t_out, gcol, tblk):
                        i = nrc[0] % nw
                        nrc[0] += 1
                        ps = banks[bi]
                        cp("act", qf[i][:], ps[:], reads=[tb[bi]], writes=[t_qf[i]])
                        act(sq[i][:], ps[:], AF.Square, reads=[tb[bi]], writes=[t_sq[i]])
                        b2 = auxring.next()
                        mm(banks[b2][:], onesb[:], sq[i][:], True, True, reads=[t_sq[i], t_c], writes=[tb[b2]])
                        act(sd[i][:], banks[b2][:], AF.Sqrt, reads=[tb[b2]], writes=[t_sd[i]], bias=EPS, scale=1.0 / 128)
                        recip(sd[i][:], sd[i][:], reads=[t_sd[i]], writes=[t_sd[i]])
                        stt(qn[i][:], qf[i][:], gqk[:, gcol:gcol + 1], sd[i][:], ALU.mult, ALU.mult,
                            reads=[t_qf[i], t_sd[i], t_tab], writes=[t_qn[i]])
                        b3 = auxring.next()
                        mm(banks[b3][:], cb[:, PERM, :], qn[i][:], True, True, reads=[t_qn[i], t_c], writes=[tb[b3]])
                        cs = slice(tblk * 512, (tblk + 1) * 512)
                        tt("dve", t1[i][:], qn[i][:], cosS[:, cs], ALU.mult, reads=[t_qn[i], t_tab], writes=[t_t1[i]])
                        tt("dve", t2[i][:], banks[b3][:], sinS[:, cs], ALU.mult, reads=[tb[b3], t_tab], writes=[t_t2[i]])
                        tt("dve", out_ap, t1[i][:], t2[i][:], ALU.add, reads=[t_t1[i], t_t2[i]], writes=[t_out])

                    def fm_block(w, tw, c0, tblk, bi):
                        for k in range(16):
                            mm(banks[bi][:], w[:, k, c0:c0 + 128], xT[:, k, tblk * 512:(tblk + 1) * 512], k == 0, k == 15,
                               reads=[tw] + t_xT[tblk * 4:(tblk + 1) * 4], writes=[tb[bi]])

                    w, tw = wring.next()
                    load_w(w, tw, [(0, w_in[:, 4128:4640])])
                    w2, tw2 = wring.next()
                    load_w(w2, tw2, [(0, w_in[:, 3104:3616])])
                    w3, tw3 = wring.next()
                    load_w(w3, tw3, [(0, w_in[:, 3616:4128])])
                    for h in range(2):
                        for blk in range(4):
                            bi = accring.next()
                            fm_block(w, tw, h * 128, blk, bi)
                            normrope(bi, kT[:, h, blk * 512:(blk + 1) * 512], t_kT[h][blk], 1, blk)
                    for t in range(NT):
                        bi = accring.next()
                        for k in range(16):
                            mm(banks[bi][:, 0:256], xT[:, k, t * 128:(t + 1) * 128], w[:, k, 256:512], k == 0, k == 15,
                               reads=[tw, t_xT[t]], writes=[tb[bi]])
                        cp("act" if t % 2 else "dve", vtok[:, t, :], banks[bi][:, 0:256], reads=[tb[bi]], writes=[t_v[t]])
                    for half, (wq, twq) in enumerate(((w2, tw2), (w3, tw3))):
                        for hh in range(4):
                            h = half * 4 + hh
                            for blk in range(4):
                                bi = accring.next()
                                fm_block(wq, twq, hh * 128, blk, bi)
                                normrope(bi, qT[:, h, blk * 512:(blk + 1) * 512], t_qT[h][blk], 0, blk)
                    if dbg and seq == 0:
                        d_q = dout("dbg_qT", [128, 8, S], BF16)
                        d_k = dout("dbg_kT", [128, 2, S], BF16)
                        d_v = dout("dbg_v", [128, NT, 256], BF16)
                        dma("sp", d_q, qT[:], reads=[t for r in t_qT for t in r])
                        dma("sp", d_k, kT[:], reads=[t for r in t_kT for t in r])
                        dma("sp", d_v, vtok[:], reads=t_v)
                    P.flush()
                    es2.close()
                    NPT = 6
                    pT = [al("pT%d" % i, [128, 512], BF16) for i in range(NPT)]
                    t_pT = P.Ts("pT", NPT)
                    rden = [al("rden%d" % i, [128, 512], F32) for i in range(2)]
                    t_rden = P.Ts("rden", 2)
                    ost = [al("ost%d" % i, [128, 512], BF16) for i in range(2)]
                    t_ost = P.Ts("ost", 2)
                    scring = Ring([0, 1, 2, 3])
                    oring = Ring([(4, 5), (6, 7)])
                    pring = Ring(list(range(NPT)))
                    PF = 3
                    t_HT = P.T("HT")
                    it = 0
                    for h in range(8):
                        g = h // 4
                        for qb in range(4):
                            bo, bd = oring.next()
                            qs = slice(qb * 512, (qb + 1) * 512)

                            def sc(kt):
                                bs = scring.next()
                                mm(banks[bs][:], kT[:, g, kt * 128:(kt + 1) * 128], qT[:, h, qs], True, True,
                                   reads=[t_kT[g][kt // 4], t_qT[h][qb]], writes=[tb[bs]])
                                return bs
                            pend = [sc(kt) for kt in range(PF)]
                            for kt in range(NT):
                                bs = pend.pop(0)
                                pi = pring.next()
                                act(pT[pi][:], banks[bs][:], AF.Exp, reads=[tb[bs]], writes=[t_pT[pi]])
                                if kt + PF < NT:
                                    pend.append(sc(kt + PF))
                                mm(banks[bo][:], vtok[:, kt, g * 128:(g + 1) * 128], pT[pi][:], kt == 0, kt == NT - 1,
                                   reads=[t_v[kt], t_pT[pi]], writes=[tb[bo]])
                                mm(banks[bd][:], onesb[:], pT[pi][:], kt == 0, kt == NT - 1,
                                   reads=[t_c, t_pT[pi]], writes=[tb[bd]])
                            i2 = it % 2
                            it += 1
                            recip(rden[i2][:], banks[bd][:], reads=[tb[bd]], writes=[t_rden[i2]])
                            tt("dve", ost[i2][:], banks[bo][:], rden[i2][:], ALU.mult, reads=[tb[bo], t_rden[i2]],
                               writes=[t_ost[i2]])
                            dma("sp", HT[seq, 1024 + h * 128: 1024 + (h + 1) * 128, qs], ost[i2][:], reads=[t_ost[i2]],
                                writes=[t_HT], partial=True)
                    P.flush()

            if "B" in stages:
                for G in range(2):
                    with ExitStack() as es:
                        al = lambda name, shape, dt: es.enter_context(nc.sbuf_tensor(uq_name(name), shape, dt))
                        qkT = al("qkT", [128, 4, S], BF16)
                        t_qk = [[P.T("qk%d_%d" % (c, t)) for t in range(4)] for c in range(4)]
                        ktok = al("ktok", [128, NT, 256], BF16)
                        t_ktok = P.Ts("ktok", NT)
                        vbase = al("vbase", [128, NT, 4, 129], BF16)
                        t_vb = P.Ts("vb", NT)
                        so = al("so", [128, NT, 512], BF16)
                        t_so = P.Ts("so", NT)
                        gsm = al("gsm", [128, NT, 4, 16], F32)
                        t_gs = P.Ts("gs", NT)
                        cw = al("cw", [128, 4, 5], F32)
                        gbias = al("gbias_sb", [128, 32], F32)
                        gml = al("gml", [128, 512], F32)
                        es2 = ExitStack()
                        al2 = lambda name, shape, dt: es2.enter_context(nc.sbuf_tensor(uq_name(name), shape, dt))
                        wsl = [al2("wb%d" % i, [128, 16, 512], BF16) for i in range(3)]
                        t_wsl = P.Ts("wb", 3)
                        wgt = al2("wgt", [128, 16, 32], BF16)
                        t_wgt = P.T("wgt")
                        t_misc = P.T("miscB")
                        dma("sp", cw[:, 0:2, :], convT[G * 256:(G + 1) * 256, :].rearrange("(c p) k -> p c k", p=128),
                            writes=[t_misc])
                        dma("sp", cw[:, 2:4, :], convT[512 + G * 256:512 + (G + 1) * 256, :].rearrange("(c p) k -> p c k", p=128),
                            writes=[t_misc], partial=True)
                        bcast_row(gbias[:], gbias_d, t_misc)
                        dma("sp", gml[:], gml_d[:, G * 512:(G + 1) * 512].partition_broadcast(128), writes=[t_misc], partial=True)
                        P.op("pool", lambda e: e.memset(vbase[:, :, :, 128:129], 1.0), writes=t_vb)
                        load_w(wsl[0], t_wsl[0], [(0, w_in[:, G * 256:(G + 1) * 256]), (256, w_in[:, 512 + G * 256:512 + (G + 1) * 256])])
                        load_w(wsl[1], t_wsl[1], [(0, w_in[:, 1024 + G * 512:1024 + (G + 1) * 512])])
                        load_w(wsl[2], t_wsl[2], [(0, w_in[:, 2048 + G * 512:2048 + (G + 1) * 512])])
                        load_w(wgt, t_wgt, [(0, w_in[:, 3072:3104])])
                        pc = [al2("pc%d" % i, [128, S + 4], BF16) for i in range(2)]
                        t_pc = P.Ts("pc", 2)
                        cacc = [al2("cacc%d" % i, [128, S], F32) for i in range(1)] * 2
                        t_cacc = P.Ts("cacc", 1) * 2
                        for i in range(2):
                            P.op("pool", lambda e, i=i: e.memset(pc[i][:, 0:2], 0.0), writes=[t_pc[i]])
                            P.op("pool", lambda e, i=i: e.memset(pc[i][:, S + 2:S + 4], 0.0), writes=[t_pc[i]], partial=True)
                        accring = Ring([0, 1, 2, 3])
                        for c in range(4):
                            i = c % 2
                            for blk in range(4):
                                bi = accring.next()
                                for k in range(16):
                                    mm(banks[bi][:], wsl[0][:, k, c * 128:(c + 1) * 128], xT[:, k, blk * 512:(blk + 1) * 512],
                                       k == 0, k == 15, reads=[t_wsl[0]] + t_xT[blk * 4:(blk + 1) * 4], writes=[tb[bi]])
                                cp("act", pc[i][:, 2 + blk * 512: 2 + (blk + 1) * 512], banks[bi][:], reads=[tb[bi]],
                                   writes=[t_pc[i]], partial=True)
                            ts("dve", cacc[i][:], pc[i][:, 0:S], cw[:, c, 0:1], ALU.mult, reads=[t_pc[i], t_misc], writes=[t_cacc[i]])
                            for j in range(1, 5):
                                stt(cacc[i][:], pc[i][:, j:j + S], cw[:, c, j:j + 1], cacc[i][:], ALU.mult, ALU.add,
                                    reads=[t_pc[i], t_misc, t_cacc[i]], writes=[t_cacc[i]])
                            for blk in range(4):
                                act(qkT[:, c, blk * 512:(blk + 1) * 512], cacc[i][:, blk * 512:(blk + 1) * 512], AF.Silu,
                                    reads=[t_cacc[i]], writes=[t_qk[c][blk]])
                        for t in range(NT):
                            bi = accring.next()
                            for pr in range(2):
                                P.op("pe", lambda e, bi=bi, pr=pr, t=t: e.transpose(
                                    bank_bf(bi)[:, pr * 128:(pr + 1) * 128], qkT[:, 2 + pr, t * 128:(t + 1) * 128], cb[:, IDENT, :]),
                                    reads=[t_qk[2 + pr][t // 4], t_c], writes=[tb[bi]], partial=pr > 0)
                            cp("act", ktok[:, t, :], bank_bf(bi)[:, 0:256], reads=[tb[bi]], writes=[t_ktok[t]])
                        zt = [al2("zt%d" % i, [128, 32], F32) for i in range(2)]
                        lf = [al2("lf%d" % i, [128, 16], F32) for i in range(2)]
                        eb = [al2("eb%d" % i, [128, 16], F32) for i in range(2)]
                        d1 = [al2("d1%d" % i, [128, 16], F32) for i in range(2)]
                        t_zt, t_lf, t_eb, t_d1 = (P.Ts(n, 2) for n in ("zt", "lf", "eb", "d1"))
                        for t in range(NT):
                            i = t % 2
                            xs_ = slice(t * 128, (t + 1) * 128)
                            bi = accring.next()
                            for k in range(16):
                                mm(banks[bi][:], xT[:, k, xs_], wsl[1][:, k, :], k == 0, k == 15, reads=[t_wsl[1], t_xT[t]],
                                   writes=[tb[bi]])
                            cp("act", vbase[:, t, :, 0:128], banks[bi][:].rearrange("p (a b) -> p a b", a=4), reads=[tb[bi]],
                               writes=[t_vb[t]], partial=True)
                            bi = accring.next()
                            for k in range(16):
                                mm(banks[bi][:], xT[:, k, xs_], wsl[2][:, k, :], k == 0, k == 15, reads=[t_wsl[2], t_xT[t]],
                                   writes=[tb[bi]])
                            act(so[:, t, :], banks[bi][:], AF.Sigmoid, reads=[tb[bi]], writes=[t_so[t]])
                            bi = accring.next()
                            for k in range(16):
                                mm(banks[bi][:, 0:32], xT[:, k, xs_], wgt[:, k, :], k == 0, k == 15, reads=[t_wgt, t_xT[t]],
                                   writes=[tb[bi]])
                            tt("dve", zt[i][:], banks[bi][:, 0:32], gbias[:], ALU.add, reads=[tb[bi], t_misc], writes=[t_zt[i]])
                            z4 = zt[i][:].rearrange("p (a b c) -> p a b c", a=2, b=2)
                            lf3 = lf[i][:].rearrange("p (a c) -> p a c", a=2)
                            act(lf3, z4[:, :, 1, :], AF.Exp, reads=[t_zt[i]], writes=[t_lf[i]], scale=-1.0)
                            act(lf[i][:], lf[i][:], AF.Ln, reads=[t_lf[i]], writes=[t_lf[i]], bias=1.0)
                            ts("dve", lf[i][:], lf[i][:], -1.0, ALU.mult, reads=[t_lf[i]], writes=[t_lf[i]])
                            bi = accring.next()
                            mm(banks[bi][:, 0:8], cf[:, MASKF, :], lf[i][:, 0:8], True, True, reads=[t_lf[i], t_c], writes=[tb[bi]])
                            mm(banks[bi][:, 8:16], cf[:, MASKB, :], lf[i][:, 8:16], True, True, reads=[t_lf[i], t_c],
                               writes=[tb[bi]], )
                            act(eb[i][:], banks[bi][:, 0:16], AF.Exp, reads=[tb[bi]], writes=[t_eb[i]])
                            ts("dve", gsm[:, t, 0, :], eb[i][:], 0.125, ALU.mult, reads=[t_eb[i]], writes=[t_gs[t]])
                            tt("dve", d1[i][:].rearrange("p (a c) -> p a c", a=2), z4[:, :, 0, :],
                               banks[bi][:, 0:16].rearrange("p (a c) -> p a c", a=2), ALU.subtract, reads=[t_zt[i], tb[bi]],
                               writes=[t_d1[i]])
                            act(gsm[:, t, 1, :], d1[i][:], AF.Exp, reads=[t_d1[i]], writes=[t_gs[t]], partial=True)
                            bi = accring.next()
                            mm(banks[bi][:, 0:8], cf[:, SELL, :], eb[i][:, 0:8], True, True, reads=[t_eb[i], t_c], writes=[tb[bi]])
                            mm(banks[bi][:, 8:16], cf[:, SELF, :], eb[i][:, 8:16], True, True, reads=[t_eb[i], t_c], writes=[tb[bi]])
                            cp("act", gsm[:, t, 3, :], banks[bi][:, 0:16], reads=[tb[bi]], writes=[t_gs[t]], partial=True)
                            tt("dve", gsm[:, t, 2, :], gsm[:, t, 1, :], banks[bi][:, 0:16], ALU.mult, reads=[tb[bi], t_gs[t]],
                               writes=[t_gs[t]], partial=True)
                        if dbg and seq == 0 and G == 0:
                            d_qk = dout("dbg_qk", [128, 4, S], BF16)
                            d_gs = dout("dbg_gs", [128, NT, 4, 16], F32)
                            d_kt = dout("dbg_ktok", [128, NT, 256], BF16)
                            dma("sp", d_qk, qkT[:], reads=[t for r in t_qk for t in r])
                            dma("sp", d_gs, gsm[:], reads=t_gs)
                            dma("sp", d_kt, ktok[:], reads=t_ktok)
                        P.flush()
                        es2.close()
                        hpart = al("hpart", [128, NT, 4, 128], BF16)
                        t_hp = [[P.T("hp%d_%d" % (t, h)) for h in range(4)] for t in range(NT)]
                        Cf = al("Cf", [128, 2, 2, 129], F32)
                        Cb = al("Cb", [128, 2, 2, 129], BF16)
                        t_C = [[[P.T("C%d%d%d" % (a, b, c)) for c in range(2)] for b in range(2)] for a in range(2)]
                        t_Cb = [[[P.T("Cb%d%d%d" % (a, b, c)) for c in range(2)] for b in range(2)] for a in range(2)]
                        P.op("dve", lambda e: e.memset(Cf[:], 0.0), writes=[t for a in t_C for b in a for t in b])
                        P.op("pool", lambda e: e.memset(Cb[:], 0.0), writes=[t for a in t_Cb for b in a for t in b])
                        NR = 4
                        sm = [al("sm%d" % i, [128, 128], BF16) for i in range(NR)]
                        v1 = [al("v1%d" % i, [128, 129], BF16) for i in range(NR)]
                        v2 = [al("v2%d" % i, [128, 129], BF16) for i in range(NR)]
                        dd = [al("dd%d" % i, [128, 2], F32) for i in range(NR)]
                        hs = [al("hs%d" % i, [128, 128], F32) for i in range(NR)]
                        hj = [al("hj%d" % i, [128, 128], F32) for i in range(NR)]
                        ssq = [al("ssq%d" % i, [128, 2], F32) for i in range(NR)]
                        t_sm, t_v1, t_v2, t_dd, t_hs, t_hj, t_ssq = (P.Ts(n, NR) for n in ("sm", "v1", "v2", "dd", "hs", "hj", "ssq"))
                        htok = [al("htok%d" % i, [128, 4, 128], BF16) for i in range(2)]
                        t_htok = P.Ts("htok", 2)
                        hTst = [al("hTst%d" % i, [128, 4, 128], BF16) for i in range(2)]
                        t_hTst = P.Ts("hTst", 2)
                        t_HT = P.T("HTb")
                        stq = Ring([0, 1])
                        oq = Ring([2, 3, 4])
                        uq = Ring([5, 6])
                        u = 0
                        done = [0] * NT
                        nfin = 0
                        LAG = 2
                        ulist = []
                        for step in range(NT):
                            for dr in range(2):
                                c = step if dr == 0 else NT - 1 - step
                                for hl in range(4):
                                    ulist.append((dr, c, hl))
                        ctx = {}
                        state = {"nfin": 0}

                        def front(uidx):
                            dr, c, hl = ulist[uidx]
                            cs = slice(c * 128, (c + 1) * 128)
                            pr, ho = hl // 2, 64 * (hl % 2)
                            rows = slice(ho, ho + 64)
                            col = dr * 8 + G * 4 + hl
                            i = uidx % NR
                            b_st = stq.next()
                            mm(banks[b_st][:, 0:128], qkT[rows, 2 + pr, cs], qkT[rows, pr, cs], True, True,
                               reads=[t_qk[2 + pr][c // 4], t_qk[pr][c // 4]], writes=[tb[b_st]])
                            tt("dve", sm[i][:], banks[b_st][:, 0:128], cf[:, MASKF if dr == 0 else MASKB, :], ALU.mult,
                               reads=[tb[b_st], t_c], writes=[t_sm[i]])
                            act(v1[i][:], vbase[:, c, hl, :], AF.Copy, reads=[t_vb[c], t_gs[c]], writes=[t_v1[i]],
                                scale=gsm[:, c, 1, col:col + 1])
                            ts("dve", v2[i][:], vbase[:, c, hl, :], gsm[:, c, 2, col:col + 1], ALU.mult,
                               reads=[t_vb[c], t_gs[c]], writes=[t_v2[i]])
                            b_o = oq.next()
                            mm(banks[b_o][:, 0:129], sm[i][:], v1[i][:], True, False, reads=[t_sm[i], t_v1[i]],
                               writes=[tb[b_o]])
                            mm(banks[b_o][:, 0:129], qkT[rows, pr, cs], Cb[rows, pr, dr, :], False, True,
                               reads=[t_qk[pr][c // 4], t_Cb[pr][dr][hl % 2]], writes=[tb[b_o]])
                            b_u = uq.next()
                            mm(banks[b_u][:, 0:129], ktok[:, c, pr * 128:(pr + 1) * 128], v2[i][:], True, True,
                               reads=[t_ktok[c], t_v2[i]], writes=[tb[b_u]])
                            tC = t_C[pr][dr][hl % 2]
                            stt(Cf[rows, pr, dr, :], Cf[rows, pr, dr, :], gsm[rows, c, 3, col:col + 1], banks[b_u][rows, 0:129],
                                ALU.mult, ALU.add, reads=[tC, t_gs[c], tb[b_u]], writes=[tC])
                            cp("act", Cb[rows, pr, dr, :], Cf[rows, pr, dr, :], reads=[tC], writes=[t_Cb[pr][dr][hl % 2]])
                            ctx[uidx] = b_o

                        def back(uidx):
                            dr, c, hl = ulist[uidx]
                            cs = slice(c * 128, (c + 1) * 128)
                            col = dr * 8 + G * 4 + hl
                            i = uidx % NR
                            b_o = ctx.pop(uidx)
                            act(dd[i][:, 0:1], banks[b_o][:, 128:129], AF.Abs, reads=[tb[b_o], t_gs[c]], writes=[t_dd[i]],
                                scale=gsm[:, c, 0, col:col + 1])
                            ts("dve", dd[i][:, 0:1], dd[i][:, 0:1], 1.0, ALU.max, reads=[t_dd[i]], writes=[t_dd[i]])
                            recip(dd[i][:, 0:1], dd[i][:, 0:1], reads=[t_dd[i]], writes=[t_dd[i]])
                            ts("dve", dd[i][:, 1:2], dd[i][:, 0:1], gsm[:, c, 0, col:col + 1], ALU.mult,
                               reads=[t_dd[i], t_gs[c]], writes=[t_dd[i]])
                            if done[c] < 4:
                                act(hpart[:, c, hl, :], banks[b_o][:, 0:128], AF.Copy, reads=[tb[b_o], t_dd[i]],
                                    writes=[t_hp[c][hl]], scale=dd[i][:, 1:2])
                            else:
                                stt(hs[i][:], banks[b_o][:, 0:128], dd[i][:, 1:2], hpart[:, c, hl, :], ALU.mult, ALU.add,
                                    reads=[tb[b_o], t_dd[i], t_hp[c][hl]], writes=[t_hs[i]])
                                act(hj[i][:], hs[i][:], AF.Square, reads=[t_hs[i]], writes=[t_hj[i], t_ssq[i]],
                                    accum_out=ssq[i][:, 0:1])
                                act(ssq[i][:, 1:2], ssq[i][:, 0:1], AF.Sqrt, reads=[t_ssq[i]], writes=[t_ssq[i]],
                                    bias=EPS, scale=1.0 / 128)
                                recip(ssq[i][:, 1:2], ssq[i][:, 1:2], reads=[t_ssq[i]], writes=[t_ssq[i]])
                                stt(hj[i][:], hs[i][:], ssq[i][:, 1:2], gml[:, hl * 128:(hl + 1) * 128], ALU.mult, ALU.mult,
                                    reads=[t_hs[i], t_ssq[i], t_misc], writes=[t_hj[i]])
                                f2 = state["nfin"] % 2
                                tt("dve", htok[f2][:, hl, :], hj[i][:], so[:, c, hl * 128:(hl + 1) * 128], ALU.mult,
                                   reads=[t_hj[i], t_so[c]], writes=[t_htok[f2]], partial=hl > 0)
                                if hl == 3:
                                    b_t = uq.next()
                                    for j in range(4):
                                        P.op("pe", lambda e, b_t=b_t, j=j, f2=f2: e.transpose(
                                            bank_bf(b_t)[:, j * 128:(j + 1) * 128], htok[f2][:, j, :], cb[:, IDENT, :]),
                                            reads=[t_htok[f2], t_c], writes=[tb[b_t]], partial=j > 0)
                                    cp("act", hTst[f2][:], bank_bf(b_t)[:, 0:512].rearrange("p (a b) -> p a b", a=4),
                                       reads=[tb[b_t]], writes=[t_hTst[f2]])
                                    dma("sp", HT[seq, G * 512:(G + 1) * 512, cs].rearrange("(a p) t -> p a t", p=128),
                                        hTst[f2][:], reads=[t_hTst[f2]], writes=[t_HT], partial=True)
                                    state["nfin"] += 1
                            done[c] += 1

                        nun = len(ulist) if "rec" not in SKIP else 0
                        for k in range(nun + LAG if nun else 0):
                            if k < nun:
                                front(k)
                            if k - LAG >= 0:
                                back(k - LAG)
                        P.flush()

        if "C" in stages:
            with ExitStack() as es:
                al = lambda name, shape, dt: es.enter_context(nc.sbuf_tensor(uq_name(name), shape, dt))
                wo = al("wo", [128, 16, D], BF16)
                wpg = al("wpg", [128, 16, D], BF16)
                wpp = al("wpp", [128, 2, D], BF16)
                wr = al("wr", [128, 16, NEXP], F32)
                lng = al("lng", [128, D], F32)
                lnb = al("lnb", [128, D], F32)
                bpgb = al("bpgb", [1, D], BF16)
                t_wo, t_wpg, t_wpp, t_wr, t_ln = P.T("wo"), P.T("wpg"), P.T("wpp"), P.T("wr"), P.T("ln1")
                for hf in range(4):
                    cs = slice(hf * 512, (hf + 1) * 512)
                    dma("pool", wo[:, :, cs], w_out[:, cs].rearrange("(c p) f -> p c f", p=128), writes=[t_wo], partial=hf > 0)
                for hf in range(4):
                    cs = slice(hf * 512, (hf + 1) * 512)
                    dma("pool", wpg[:, :, cs], wpg_d[:, cs].rearrange("(c p) f -> p c f", p=128), writes=[t_wpg], partial=hf > 0)
                dma("pool", wpp[:], wpp_d.rearrange("(c p) f -> p c f", p=128), writes=[t_wpp])
                if "bpgb" not in SKIP:
                    dma("pool", bpgb[:], bpg_d, writes=[t_wpp], partial=True)
                dma("sp", wr[:], wr_d.rearrange("(c p) f -> p c f", p=128), writes=[t_wr])
                dma("sp", lng[:], ln1g_d.partition_broadcast(128), writes=[t_ln])
                dma("sp", lnb[:], ln1b_d.partition_broadcast(128), writes=[t_ln], partial=True)
                hTt = al("hTt", [128, 16, 128], BF16)
                xt = al("xt", [128, D], F32)
                pt = al("pt", [128, 256], F32)
                x1b = al("x1b", [128, D], BF16)
                x1Tf = al("x1Tf", [128, 16, 128], F32)
                x1Tb = al("x1Tb", [128, 16, 128], BF16)
                pTb = al("pTb", [128, 2, 128], BF16)
                sg = al("sg", [128, 512], F32)
                plw = al("plw", [128, 512], F32)
                lnw = al("lnw", [128, 32], F32)
                rt = al("rt", [128, 24], F32)
                affp = al("affp", [128, 64], F32)
                t_hTt, t_xt, t_pt, t_x1b, t_x1Tf, t_x1Tb, t_pTb, t_sg, t_plw, t_lnw, t_rt, t_affp = (
                    P.T(n) for n in ("hTt", "xt", "pt", "x1b", "x1Tf", "x1Tb", "pTb", "sg", "plw", "lnw", "rt", "affp"))
                t_X1B, t_R = P.T("X1B"), P.T("Rw")
                P.op("dve", lambda e: e.memset(affp[:], 0.0), writes=[t_affp])
                bring = Ring(list(range(8)))
                for t in range(NT):
                    rows = slice(tok0 + t * 128, tok0 + (t + 1) * 128)
                    dma("sp", hTt[:], HT[seq].rearrange("(c p) t -> p c t", p=128)[:, :, t * 128:(t + 1) * 128], writes=[t_hTt])
                    dma("sp", xt[:], x_d[rows, :], writes=[t_xt])
                    dma("sp", pt[:], p_d[rows, :], writes=[t_pt])
                    for dg in range(4):
                        bi = bring.next()
                        cs = slice(dg * 512, (dg + 1) * 512)
                        for k in range(16):
                            mm(banks[bi][:], hTt[:, k, :], wo[:, k, cs], k == 0, k == 15, reads=[t_hTt, t_wo], writes=[tb[bi]])
                        stt(xt[:, cs], xt[:, cs], ALPHA, banks[bi][:], ALU.mult, ALU.add, reads=[t_xt, tb[bi]], writes=[t_xt],
                            partial=dg > 0)
                    if "ln" not in SKIP:
                        layernorm(xt, t_xt, lng, lnb, t_ln, lnw, t_lnw)
                    if "x1b" not in SKIP:
                        cp("act", x1b[:], xt[:], reads=[t_xt], writes=[t_x1b])
                        dma("sp", X1B[rows, :], x1b[:], reads=[t_x1b], writes=[t_X1B], partial=True)
                    for g in range(4 if "xtr" not in SKIP else 0):
                        bi = bring.next()
                        for j in range(4):
                            c = g * 4 + j
                            P.op("pe", lambda e, bi=bi, j=j, c=c: e.transpose(
                                banks[bi][:, j * 128:(j + 1) * 128], xt[:, c * 128:(c + 1) * 128], cf[:, IDENT, :]),
                                reads=[t_xt, t_c], writes=[tb[bi]], partial=j > 0)
                        pv = banks[bi][:].rearrange("p (a b) -> p a b", a=4)
                        cp("act", x1Tf[:, g * 4:(g + 1) * 4, :], pv, reads=[tb[bi]], writes=[t_x1Tf], partial=g > 0)
                        if "x1tb" not in SKIP:
                            cp("dve", x1Tb[:, g * 4:(g + 1) * 4, :], x1Tf[:, g * 4:(g + 1) * 4, :], reads=[t_x1Tf], writes=[t_x1Tb],
                               partial=g > 0)
                    if "router" not in SKIP:
                        bi = bring.next()
                        for k in range(16):
                            mm(banks[bi][:, 0:NEXP], x1Tf[:, k, :], wr[:, k, :], k == 0, k == 15, reads=[t_x1Tf, t_wr], writes=[tb[bi]])
                        P.op("dve", lambda e, bi=bi: e.tensor_reduce(out=rt[:, 16:17], in_=banks[bi][:, 0:NEXP],
                                                                      axis=mybir.AxisListType.X, op=ALU.max, negate=True),
                             reads=[tb[bi]], writes=[t_rt])
                        act(rt[:, 0:NEXP], banks[bi][:, 0:NEXP], AF.Exp, reads=[tb[bi], t_rt], writes=[t_rt], bias=rt[:, 16:17],
                            accum_out=rt[:, 17:18])
                        recip(rt[:, 18:19], rt[:, 17:18], reads=[t_rt], writes=[t_rt])
                        ts("dve", affp[:, 32 * seq:32 * seq + NEXP], rt[:, 0:NEXP], rt[:, 18:19], ALU.mult, reads=[t_rt],
                           writes=[t_affp])
                        bi = bring.next()
                        mm(banks[bi][0:64, 0:128], affp[:], cf[:, IDENT, :], True, True, reads=[t_affp, t_c], writes=[tb[bi]])
                        cp("act", affT[32 * seq:32 * seq + NEXP, t * 128:(t + 1) * 128], banks[bi][32 * seq:32 * seq + NEXP, 0:128],
                           reads=[tb[bi]], writes=[t_affT], partial=True)
                    if "pl" not in SKIP:
                        bi = bring.next()
                        for j in range(2):
                            P.op("pe", lambda e, bi=bi, j=j: e.transpose(
                                banks[bi][:, j * 128:(j + 1) * 128], pt[:, j * 128:(j + 1) * 128], cf[:, IDENT, :]),
                                reads=[t_pt, t_c], writes=[tb[bi]], partial=j > 0)
                        cp("act", pTb[:], banks[bi][:, 0:256].rearrange("p (a b) -> p a b", a=2), reads=[tb[bi]], writes=[t_pTb])
                        for dg in range(4):
                            cs = slice(dg * 512, (dg + 1) * 512)
                            bi = bring.next()
                            for k in range(16):
                                mm(banks[bi][:], x1Tb[:, k, :], wpg[:, k, cs], k == 0, False, reads=[t_x1Tb, t_wpg], writes=[tb[bi]])
                            mm(banks[bi][:], onesb[0:1, :], bpgb[0:1, cs], False, True, reads=[t_c, t_wpp], writes=[tb[bi]])
                            act(sg[:], banks[bi][:], AF.Sigmoid, reads=[tb[bi]], writes=[t_sg])
                            b2 = bring.next()
                            for k in range(2):
                                mm(banks[b2][:], pTb[:, k, :], wpp[:, k, cs], k == 0, k == 1, reads=[t_pTb, t_wpp], writes=[tb[b2]])
                            tt("dve", plw[:], sg[:], banks[b2][:], ALU.mult, reads=[t_sg, tb[b2]], writes=[t_plw])
                            stt(xt[:, cs], xt[:, cs], ALPHA, plw[:], ALU.mult, ALU.add, reads=[t_xt, t_plw], writes=[t_xt])
                    dma("sp", R[rows, :], xt[:], reads=[t_xt], writes=[t_R], partial=True)
                P.flush()

    if "M" in stages:
        with ExitStack() as es:
            al = lambda name, shape, dt: es.enter_context(nc.sbuf_tensor(uq_name(name), shape, dt))
            NC_ = 2 * nseq
            NCOL = 128 * NC_
            tkw = al("tkw", [64, S], F32)
            top = al("top", [64, CAP], F32)
            idxu = al("idxu", [64, CAP], U32)
            idxf = al("idxf", [64, CAP], F32)
            idxT = al("idxT", [128, 2, 64], I32)
            gateT = al("gateT", [128, 2, 64], F32)
            t_tk, t_top, t_idxu, t_idxf, t_idxT, t_gateT = (P.T(n) for n in ("tkw", "top", "idxu", "idxf", "idxT", "gateT"))
            cp("dve", tkw[:], affT[:], reads=[t_affT], writes=[t_tk])
            for r in range(CAP // 8):
                rs = slice(r * 8, (r + 1) * 8)
                P.op("dve", lambda e, rs=rs: e.max(out=top[:, rs], in_=tkw[:]), reads=[t_tk], writes=[t_top], partial=True)
                P.op("dve", lambda e, rs=rs: e.max_index(out=idxu[:, rs], in_max=top[:, rs], in_values=tkw[:]),
                     reads=[t_tk, t_top], writes=[t_idxu], partial=True)
                P.op("dve", lambda e, rs=rs: e.match_replace(out=tkw[:], in_to_replace=top[:, rs], in_values=tkw[:], imm_value=-1.0),
                     reads=[t_top, t_tk], writes=[t_tk])
            cp("dve", idxf[:], idxu[:], reads=[t_idxu], writes=[t_idxf])
            if nseq > 1:
                ts("dve", idxf[32:48, :], idxf[32:48, :], float(S), ALU.add, reads=[t_idxf], writes=[t_idxf])
            for ct in range(2):
                P.op("pe", lambda e, ct=ct: e.transpose(banks[0][:, ct * 64:(ct + 1) * 64], idxf[:, ct * 128:(ct + 1) * 128],
                                                      cf[0:64, IDENT, 0:64]), reads=[t_idxf, t_c], writes=[tb[0]], partial=ct > 0)
                P.op("pe", lambda e, ct=ct: e.transpose(banks[1][:, ct * 64:(ct + 1) * 64], top[:, ct * 128:(ct + 1) * 128],
                                                      cf[0:64, IDENT, 0:64]), reads=[t_top, t_c], writes=[tb[1]], partial=ct > 0)
            cp("dve", idxT[:], banks[0][:, 0:128].rearrange("p (a b) -> p a b", a=2), reads=[tb[0]], writes=[t_idxT])
            cp("act", gateT[:], banks[1][:, 0:128].rearrange("p (a b) -> p a b", a=2), reads=[tb[1]], writes=[t_gateT])
            if dbg:
                d_idx = dout("dbg_idxT", [128, 2, 64], I32)
                d_gate = dout("dbg_gateT", [128, 2, 64], F32)
                dma("sp", d_idx, idxT[:], reads=[t_idxT])
                dma("sp", d_gate, gateT[:], reads=[t_gateT])
            NSL = 7
            wsl = [al("wm%d" % i, [128, 8192], BF16) for i in range(NSL)]
            t_wsl = P.Ts("wm", NSL)
            xs = [al("xs%d" % i, [128, D], BF16) for i in range(NC_)]
            t_xs = P.Ts("xs", NC_)
            xsT = al("xsT", [128, 16, NCOL], BF16)
            t_xsT = P.Ts("xsT", NC_)
            hT = al("hT", [128, 8, NCOL], BF16)
            t_hT = P.Ts("hT", 8)
            sgw = [al("sgw%d" % i, [128, NCOL], F32) for i in range(2)]
            t_sgw = P.Ts("sgw", 2)
            yst = [al("yst%d" % i, [128, D], F32) for i in range(2)]
            t_yst = P.Ts("yst", 2)
            t_Rm = P.T("Rm")
            t_X1Bm = P.T("X1Bm")

            def piece_ap(n):
                e, i = divmod(n, 6)
                if i < 4:
                    src = (wg_d if i % 2 == 0 else wu_d)[e][:, (i // 2) * 512:(i // 2 + 1) * 512]
                    return src.rearrange("(c p) f -> p c f", p=128), "p (k f) -> p k f", 16
                hh = i - 4
                return wd_d[e][:, hh * 1024:(hh + 1) * 1024].rearrange("(c p) f -> p c f", p=128), "p (k f) -> p k f", 8

            def load_piece(n):
                if n >= 6 * NEXP:
                    return
                src, pat, kk = piece_ap(n)
                sl = n % NSL
                dma("pool", wsl[sl][:].rearrange(pat, k=kk), src, writes=[t_wsl[sl]])

            def wview(n, kk):
                return wsl[n % NSL][:].rearrange("p (k f) -> p k f", k=kk), t_wsl[n % NSL]

            for n in range(NSL):
                load_piece(n)
            bring = Ring(list(range(8)))
            sgi = 0
            for e in range(NEXP):
                cols = []
                for j in range(NC_):
                    sq_, ct = divmod(j, 2)
                    col = 32 * sq_ + e
                    cols.append((ct, col))
                    P.op("pool", lambda eng, j=j, ct=ct, col=col: eng.indirect_dma_start(
                        out=xs[j][:], out_offset=None, in_=X1B,
                        in_offset=bass.IndirectOffsetOnAxis(ap=idxT[:, ct, col:col + 1], axis=0)),
                        reads=[t_idxT, t_X1Bm], writes=[t_xs[j]], dma=True)
                for j in range(NC_):
                    for g4 in range(4):
                        bi = bring.next()
                        for q4 in range(4):
                            c = g4 * 4 + q4
                            P.op("pe", lambda eng, bi=bi, q4=q4, c=c, j=j: eng.transpose(
                                bank_bf(bi)[:, q4 * 128:(q4 + 1) * 128], xs[j][:, c * 128:(c + 1) * 128], cb[:, IDENT, :]),
                                reads=[t_xs[j], t_c], writes=[tb[bi]], partial=q4 > 0)
                        cp("act" if g4 % 2 else "dve", xsT[:, g4 * 4:(g4 + 1) * 4, j * 128:(j + 1) * 128],
                           bank_bf(bi)[:, 0:512].rearrange("p (a b) -> p a b", a=4), reads=[tb[bi]], writes=[t_xsT[j]],
                           partial=g4 > 0)
                for fh in range(2):
                    ng, nu = 6 * e + 2 * fh, 6 * e + 2 * fh + 1
                    wgv, twg = wview(ng, 16)
                    wuv, twu = wview(nu, 16)
                    for ft in range(4):
                        f = fh * 4 + ft
                        bg = bring.next()
                        for k in range(16):
                            mm(banks[bg][:, 0:NCOL], wgv[:, k, ft * 128:(ft + 1) * 128], xsT[:, k, :], k == 0, k == 15,
                               reads=[twg] + t_xsT, writes=[tb[bg]])
                        bu = bring.next()
                        for k in range(16):
                            mm(banks[bu][:, 0:NCOL], wuv[:, k, ft * 128:(ft + 1) * 128], xsT[:, k, :], k == 0, k == 15,
                               reads=[twu] + t_xsT, writes=[tb[bu]])
                        si = sgi % 2
                        sgi += 1
                        act(sgw[si][:], banks[bg][:, 0:NCOL], AF.Silu, reads=[tb[bg]], writes=[t_sgw[si]])
                        tt("dve", hT[:, f, :], sgw[si][:], banks[bu][:, 0:NCOL], ALU.mult, reads=[t_sgw[si], tb[bu]],
                           writes=[t_hT[f]])
                    load_piece(ng + NSL)
                    load_piece(nu + NSL)
                nd0, nd1 = 6 * e + 4, 6 * e + 5
                for j in range(NC_):
                    ct, col = cols[j]
                    yi = (e * NC_ + j) % 2
                    for dh, nd in enumerate((nd0, nd1)):
                        wdv, twd = wview(nd, 8)
                        for dg2 in range(2):
                            bi = bring.next()
                            for k in range(8):
                                mm(banks[bi][:], hT[:, k, j * 128:(j + 1) * 128], wdv[:, k, dg2 * 512:(dg2 + 1) * 512], k == 0, k == 7,
                                   reads=[t_hT[k], twd], writes=[tb[bi]])
                            oc = dh * 1024 + dg2 * 512
                            act(yst[yi][:, oc:oc + 512], banks[bi][:], AF.Copy, reads=[tb[bi], t_gateT], writes=[t_yst[yi]],
                                scale=gateT[:, ct, col:col + 1], partial=(dh + dg2) > 0)
                    P.op("pool", lambda eng, yi=yi, ct=ct, col=col: eng.indirect_dma_start(
                        out=R, out_offset=bass.IndirectOffsetOnAxis(ap=idxT[:, ct, col:col + 1], axis=0),
                        in_=yst[yi][:], in_offset=None, compute_op=ALU.add),
                        reads=[t_idxT, t_yst[yi]], writes=[t_Rm], dma=True)
                load_piece(nd0 + NSL)
                load_piece(nd1 + NSL)
            P.flush()

    if "F" in stages:
        with ExitStack() as es:
            al = lambda name, shape, dt: es.enter_context(nc.sbuf_tensor(uq_name(name), shape, dt))
            lng = al("ln2g", [128, D], F32)
            lnb = al("ln2b", [128, D], F32)
            t_ln = P.T("ln2")
            dma("sp", lng[:], ln2g_d.partition_broadcast(128), writes=[t_ln])
            dma("sp", lnb[:], ln2b_d.partition_broadcast(128), writes=[t_ln], partial=True)
            NB = 4
            rt_ = [al("rf%d" % i, [128, D], F32) for i in range(NB)]
            lw = [al("lw%d" % i, [128, 32], F32) for i in range(NB)]
            t_rt_ = P.Ts("rf", NB)
            t_lw = P.Ts("lw", NB)
            t_out = P.T("out")
            for t in range(nseq * NT):
                i = t % NB
                rows = slice(t * 128, (t + 1) * 128)
                dma("sp", rt_[i][:], R[rows, :], writes=[t_rt_[i]])
                layernorm(rt_[i], t_rt_[i], lng, lnb, t_ln, lw[i], t_lw[i])
                dma("sp", out_d[rows, :], rt_[i][:], reads=[t_rt_[i]], writes=[t_out], partial=True)
    P.flush(final=True)
    return nc, dbg_out, P


def _consts():
    c = np.zeros((6, 128, 128), np.float32)
    i = np.arange(128)
    c[0] = np.eye(128, dtype=np.float32)
    c[1] = (i[:, None] <= i[None, :]).astype(np.float32)
    c[2] = (i[:, None] >= i[None, :]).astype(np.float32)
    c[3][127, :] = 1.0
    c[4][0, :] = 1.0
    partner = np.where((i % 64) < 32, i + 32, i - 32)
    c[5][partner, i] = 1.0
    t = np.arange(S)
    row = (t // 64).astype(np.float32)
    colp = (t % 64).astype(np.float32)
    inv = (np.float32(10000.0) ** (-np.arange(32, dtype=np.float32) / np.float32(32))).astype(np.float32)
    cosT = np.zeros((128, S), np.float32)
    sinT = np.zeros((128, S), np.float32)
    for d in range(128):
        pos = row if d < 64 else colp
        ang = (pos * inv[d % 32]).astype(np.float32)
        cosT[d] = np.cos(ang)
        sgn = -1.0 if (d % 64) < 32 else 1.0
        sinT[d] = sgn * np.sin(ang)
    return c, cosT, sinT


def prep_shared(inp):
    f = lambda a: np.ascontiguousarray(np.asarray(a, dtype=np.float32))
    c, cosT, sinT = _consts()
    bi = f(inp["b_igate"])[0]
    bf = f(inp["b_fgate"])[0]
    gb = np.stack([bi, bf], axis=1).reshape(1, 32)
    return {
        "w_in": f(inp["w_in"])[0], "convT": f(np.asarray(inp["conv_w"])[0].T), "gbias": f(gb),
        "g_mlstm": f(inp["g_mlstm"]).reshape(1, 1024), "g_q": f(inp["g_q"]).reshape(128, 1),
        "g_k": f(inp["g_k"]).reshape(128, 1), "w_out": f(inp["w_out"])[0],
        "ln1_g": f(inp["ln1_g"]).reshape(1, D), "ln1_b": f(inp["ln1_b"]).reshape(1, D),
        "w_router": f(inp["w_router"])[0], "w_gate": f(inp["w_gate"])[0], "w_up": f(inp["w_up"])[0],
        "w_down": f(inp["w_down"])[0], "w_pl_proj": f(inp["w_pl_proj"])[0], "w_pl_gate": f(inp["w_pl_gate"])[0],
        "b_pl_gate": f(inp["b_pl_gate"]).reshape(1, D), "ln2_g": f(inp["ln2_g"]).reshape(1, D),
        "ln2_b": f(inp["ln2_b"]).reshape(1, D), "cst": c, "cosT": cosT, "sinT": sinT,
    }


_CACHE = {}


def kernel(**inputs):
    ncores = 8
    sh = prep_shared(inputs)
    x = np.asarray(inputs["x"], dtype=np.float32)
    p = np.asarray(inputs["p"], dtype=np.float32)[0]
    nseq = x.shape[0] // ncores
    if "nc" not in _CACHE:
        _CACHE["nc"] = build(nseq=nseq, stages="ABCMF", dbg=False)[0]
    nc = _CACHE["nc"]
    in_maps = []
    for c in range(ncores):
        m = dict(sh)
        m["x"] = np.ascontiguousarray(x[c * nseq:(c + 1) * nseq].reshape(nseq * S, D))
        m["p"] = np.ascontiguousarray(p[c * nseq:(c + 1) * nseq].reshape(nseq * S, 256))
        in_maps.append(m)
    res = run_bass_kernel_spmd(nc, in_maps, core_ids=list(range(ncores)))
    out = np.concatenate([np.asarray(r["out"]).reshape(nseq, S, D) for r in res.results], axis=0)
    return out.astype(np.float32)
```

```python
import math
from contextlib import ExitStack
import numpy as np
import concourse.bass as bass
import concourse.mybir as mybir
from concourse.alu_op_type import AluOpType as ALU
from concourse.bass_utils import run_bass_kernel_spmd

AF = mybir.ActivationFunctionType
F32 = mybir.dt.float32
BF16 = mybir.dt.bfloat16
I32 = mybir.dt.int32
U32 = mybir.dt.uint32

ENGS = ("pe", "act", "dve", "pool", "sp")
D = 2048
S = 2048
NT = 16
NEXP = 16
CAP = 256
FF = 1024
EPS = 1e-6
ALPHA = 2.0 ** 0.25


class T:
    __slots__ = ("name", "w", "r")

    def __init__(self, name):
        self.name = name
        self.w = []
        self.r = []


class Op:
    __slots__ = ("idx", "eng", "fn", "deps", "dma", "flag", "evt")

    def __init__(self, idx, eng, fn, deps, dma):
        self.idx = idx
        self.eng = eng
        self.fn = fn
        self.deps = deps
        self.dma = dma
        self.flag = False
        self.evt = None


class Prog:
    def __init__(self, nc, n_dma_sems=32):
        self.nc = nc
        self.ops = []
        self.q = {e: [] for e in ENGS}
        self.tiles = []
        self.nd = n_dma_sems
        self.esem = {e: nc.alloc_semaphore("s_" + e) for e in ENGS}
        self.dsems = [nc.alloc_semaphore("d%d" % i) for i in range(n_dma_sems)]
        self.dval = [0] * n_dma_sems
        self.dnext = {"sp": 0, "pool": 0}
        self.cnt = {e: 0 for e in ENGS}
        self.di = 0
        self.known = {e: {} for e in ENGS}
        self.nins = 0

    def T(self, name):
        t = T(name)
        self.tiles.append(t)
        return t

    def Ts(self, name, n):
        return [self.T("%s%d" % (name, i)) for i in range(n)]

    def op(self, eng, fn, reads=(), writes=(), dma=False, partial=False):
        idx = len(self.ops)
        deps = set()
        for t in reads:
            deps.update(t.w)
        for t in writes:
            deps.update(t.w)
            deps.update(t.r)
        o = Op(idx, eng, fn, deps, dma)
        for t in reads:
            t.r.append(idx)
        for t in writes:
            if partial:
                t.w.append(idx)
            else:
                t.w = [idx]
            t.r = []
        self.ops.append(o)
        self.q[eng].append(o)
        return o

    def flush(self, final=False):
        nc = self.nc
        ops = self.ops
        alld = set(o.idx for o in ops if o.dma)
        for e in ENGS:
            if self.q[e]:
                alld.add(self.q[e][-1].idx)
        for e in ENGS:
            o = Op(len(ops), e, lambda eng: eng.nop(), set(alld), False)
            ops.append(o)
            self.q[e].append(o)
        for o in ops:
            best = {}
            keep = set()
            for d in o.deps:
                p = ops[d]
                if p.dma:
                    keep.add(d)
                    continue
                if p.eng == "pe" and o.eng == "pe" and not o.dma:
                    continue
                b = best.get(p.eng)
                if b is None or d > b:
                    best[p.eng] = d
            keep.update(best.values())
            o.deps = keep
            for d in keep:
                ops[d].flag = True
        dma_prev = {}
        for o in ops:
            if o.dma:
                half = self.nd // 2
                s = (self.dnext[o.eng] % half) + (0 if o.eng == "sp" else half)
                self.dnext[o.eng] += 1
                dma_prev[o.idx] = (self.dsems[s], self.dval[s])
                self.dval[s] += 16
                o.evt = (self.dsems[s], self.dval[s])
            elif o.flag:
                self.cnt[o.eng] += 1
                o.evt = (self.esem[o.eng], self.cnt[o.eng])
        finals = [(self.dsems[i], self.dval[i]) for i in range(self.nd) if self.dval[i]]

        def run_queue(ename, eng):
            known = self.known[ename]
            for o in self.q[ename]:
                need = {}
                ws = []
                if o.dma:
                    ws.append(dma_prev[o.idx])
                for d in o.deps:
                    ws.append(ops[d].evt)
                for (s, v) in ws:
                    if v <= 0:
                        continue
                    k = id(s)
                    if known.get(k, 0) >= v:
                        continue
                    if k not in need or need[k][1] < v:
                        need[k] = (s, v)
                for k, (s, v) in need.items():
                    known[k] = v
                    eng.wait_ge(s, v)
                    self.nins += 1
                ins = o.fn(eng)
                self.nins += 1
                if o.dma:
                    ins.then_inc(o.evt[0], 16)
                elif o.flag:
                    ins.then_inc(o.evt[0], 1)
            if final and ename == "sp":
                for (s, v) in finals:
                    eng.wait_ge(s, v)

        with nc.Block() as block:
            @block.tensor
            def _(e):
                run_queue("pe", e)

            @block.scalar
            def _(e):
                run_queue("act", e)

            @block.vector
            def _(e):
                run_queue("dve", e)

            @block.gpsimd
            def _(e):
                run_queue("pool", e)

            @block.sync
            def _(e):
                run_queue("sp", e)
        self.ops = []
        self.q = {e: [] for e in ENGS}
        for t in self.tiles:
            t.w = []
            t.r = []


class Ring:
    def __init__(self, items):
        self.items = items
        self.i = 0

    def next(self):
        it = self.items[self.i % len(self.items)]
        self.i += 1
        return it


def build(nseq=2, stages="ABCMF", dbg=False):
    import os
    SKIP = set(os.environ.get("KSKIP", "").split(","))
    nc = bass.Bass("TRN2", target_bir_lowering=False)
    NTOK = nseq * S
    P = Prog(nc)

    def din(name, shape, dt=F32):
        return nc.dram_tensor(name, list(shape), dt, kind="ExternalInput").ap()

    x_d = din("x", [NTOK, D])
    p_d = din("p", [NTOK, 256])
    w_in = din("w_in", [D, 4640])
    convT = din("convT", [1024, 5])
    gbias_d = din("gbias", [1, 32])
    gml_d = din("g_mlstm", [1, 1024])
    gq_d = din("g_q", [128, 1])
    gk_d = din("g_k", [128, 1])
    w_out = din("w_out", [D, D])
    ln1g_d = din("ln1_g", [1, D])
    ln1b_d = din("ln1_b", [1, D])
    wr_d = din("w_router", [D, NEXP])
    wg_d = din("w_gate", [NEXP, D, FF])
    wu_d = din("w_up", [NEXP, D, FF])
    wd_d = din("w_down", [NEXP, FF, D])
    wpp_d = din("w_pl_proj", [256, D])
    wpg_d = din("w_pl_gate", [D, D])
    bpg_d = din("b_pl_gate", [1, D])
    ln2g_d = din("ln2_g", [1, D])
    ln2b_d = din("ln2_b", [1, D])
    cst_d = din("cst", [6, 128, 128])
    cos_d = din("cosT", [128, S])
    sin_d = din("sinT", [128, S])
    out_d = nc.dram_tensor("out", [NTOK, D], F32, kind="ExternalOutput").ap()
    ikind = "ExternalOutput" if dbg else "Internal"
    HT = nc.dram_tensor("HT", [nseq, D, S], BF16, kind=ikind).ap()
    X1B = nc.dram_tensor("X1B", [NTOK, D], BF16, kind=ikind).ap()
    R = nc.dram_tensor("R", [NTOK, D], F32, kind=ikind).ap()
    dbg_out = {}
    _uq = [0]

    def uq_name(n):
        _uq[0] += 1
        return "%s_u%d" % (n, _uq[0])

    def dout(name, shape, dt=F32):
        a = nc.dram_tensor(name, list(shape), dt, kind="ExternalOutput").ap()
        dbg_out[name] = a
        return a

    cf = nc.alloc_sbuf_tensor("cf", [128, 6, 128], F32)
    cb = nc.alloc_sbuf_tensor("cb", [128, 6, 128], BF16)
    onesb = nc.alloc_sbuf_tensor("onesb", [128, 128], BF16)
    t_c = P.T("consts")
    IDENT, MASKF, MASKB, SELL, SELF, PERM = range(6)
    P.op("sp", lambda e: e.dma_start(out=cf[:], in_=cst_d.rearrange("c p f -> p c f")), writes=[t_c], dma=True)
    P.op("dve", lambda e: e.tensor_copy(out=cb[:], in_=cf[:]), reads=[t_c], writes=[t_c], partial=True)
    P.op("dve", lambda e: e.memset(onesb[:], 1.0), writes=[t_c], partial=True)

    banks = [nc.alloc_psum_tensor("bank%d" % i, [128, 512], F32) for i in range(8)]
    tb = P.Ts("bank", 8)

    def bank_bf(i):
        return banks[i][:].bitcast(BF16)

    def dma(q, out, in_, reads=(), writes=(), partial=False):
        return P.op(q, lambda e: e.dma_start(out=out, in_=in_), reads=reads, writes=writes, dma=True, partial=partial)

    def mm(out, lhsT, rhs, start, stop, reads, writes):
        return P.op("pe", lambda e: e.matmul(out, lhsT, rhs, start=start, stop=stop), reads=reads, writes=writes,
                    partial=not start)

    def act(out, in_, func, reads, writes, bias=None, scale=None, accum_out=None, partial=False):
        kw = {}
        if bias is not None:
            kw["bias"] = bias
        if scale is not None:
            kw["scale"] = scale
        if accum_out is not None:
            kw["accum_out"] = accum_out
        return P.op("act", lambda e: e.activation(out, in_, func, **kw), reads=reads, writes=writes, partial=partial)

    def ts(eng, out, in0, s1, op0, reads, writes, s2=None, op1=None, partial=False):
        if op1 is None:
            return P.op(eng, lambda e: e.tensor_scalar(out, in0, s1, None, op0), reads=reads, writes=writes, partial=partial)
        return P.op(eng, lambda e: e.tensor_scalar(out, in0, s1, s2, op0, op1), reads=reads, writes=writes, partial=partial)

    def tt(eng, out, in0, in1, op, reads, writes, partial=False):
        return P.op(eng, lambda e: e.tensor_tensor(out, in0, in1, op), reads=reads, writes=writes, partial=partial)

    def stt(out, in0, scalar, in1, op0, op1, reads, writes, partial=False):
        return P.op("dve", lambda e: e.scalar_tensor_tensor(out, in0, scalar, in1, op0, op1), reads=reads, writes=writes,
                    partial=partial)

    def cp(eng, out, in_, reads, writes, partial=False):
        if eng == "act":
            return P.op("act", lambda e: e.copy(out, in_), reads=reads, writes=writes, partial=partial)
        return P.op(eng, lambda e: e.tensor_copy(out=out, in_=in_), reads=reads, writes=writes, partial=partial)

    def recip(out, in_, reads, writes):
        return P.op("dve", lambda e: e.reciprocal(out, in_), reads=reads, writes=writes)

    def load_w(slot, tslot, pieces):
        first = True
        for (c0, ap) in pieces:
            n = ap.shape[1]
            kc = ap.shape[0] // 128
            dma("pool", slot[:, 0:kc, c0:c0 + n], ap.rearrange("(c p) f -> p c f", p=128), writes=[tslot],
                partial=not first)
            first = False

    def bcast_row(dst, row_ap, tdst):
        dma("sp", dst, row_ap.partition_broadcast(128), writes=[tdst])

    affT = nc.alloc_sbuf_tensor("affT", [64, S], F32)
    t_affT = P.T("affT")
    P.op("pool", lambda e: e.memset(affT[:], 0.0), writes=[t_affT])

    def layernorm(x, t_x, g, b, t_gb, w, t_w):
        st = w[:, 0:24].rearrange("p (a b) -> p a b", a=4)
        for c in range(4):
            P.op("dve", lambda e, c=c: e.bn_stats(out=st[:, c, :], in_=x[:, c * 512:(c + 1) * 512]), reads=[t_x], writes=[t_w],
                 partial=c > 0)
        P.op("dve", lambda e: e.bn_aggr(out=w[:, 24:26], in_=w[:, 0:24]), reads=[t_w], writes=[t_w])
        act(w[:, 26:27], w[:, 25:26], AF.Sqrt, reads=[t_w], writes=[t_w], bias=EPS, scale=1.0)
        recip(w[:, 26:27], w[:, 26:27], reads=[t_w], writes=[t_w])
        ts("dve", x[:], x[:], w[:, 24:25], ALU.subtract, reads=[t_x, t_w], writes=[t_x], s2=w[:, 26:27], op1=ALU.mult)
        tt("dve", x[:], x[:], g[:], ALU.mult, reads=[t_x, t_gb], writes=[t_x])
        tt("dve", x[:], x[:], b[:], ALU.add, reads=[t_x, t_gb], writes=[t_x])

    P.flush()

    for seq in range(nseq):
        tok0 = seq * S
        with ExitStack() as seq_es:
            if any(c in stages for c in "AB"):
                xT = seq_es.enter_context(nc.sbuf_tensor(uq_name("xT"), [128, 16, S], BF16))
                t_xT = P.Ts("xT", NT)
                with ExitStack() as es:
                    xin = [es.enter_context(nc.sbuf_tensor(uq_name("xin%d" % i), [128, D], F32)) for i in range(2)]
                    t_xin = P.Ts("xin", 2)
                    k = 0
                    for t in range(NT):
                        b = t % 2
                        dma("sp", xin[b][:], x_d[tok0 + t * 128: tok0 + (t + 1) * 128, :], writes=[t_xin[b]])
                        for g in range(4):
                            bi = k % 4
                            k += 1
                            for j in range(4):
                                c = g * 4 + j
                                P.op("pe", lambda e, bi=bi, j=j, b=b, c=c: e.transpose(
                                    banks[bi][:, j * 128:(j + 1) * 128], xin[b][:, c * 128:(c + 1) * 128], cf[:, IDENT, :]),
                                    reads=[t_xin[b], t_c], writes=[tb[bi]], partial=j > 0)
                            cp("act" if g % 2 == 0 else "dve", xT[:, g * 4:(g + 1) * 4, t * 128:(t + 1) * 128],
                               banks[bi][:].rearrange("p (a b) -> p a b", a=4), reads=[tb[bi]], writes=[t_xT[t]],
                               partial=g > 0)
                    P.flush()

            if "A" in stages:
                with ExitStack() as es:
                    al = lambda name, shape, dt: es.enter_context(nc.sbuf_tensor(uq_name(name), shape, dt))
                    qT = al("qT", [128, 8, S], BF16)
                    kT = al("kT", [128, 2, S], BF16)
                    vtok = al("vtok", [128, NT, 256], BF16)
                    cosS = al("cosS", [128, S], F32)
                    sinS = al("sinS", [128, S], F32)
                    gqk = al("gqk", [128, 2], F32)
                    es2 = ExitStack()
                    al2 = lambda name, shape, dt: es2.enter_context(nc.sbuf_tensor(uq_name(name), shape, dt))
                    wsl = [al2("wa%d" % i, [128, 16, 512], BF16) for i in range(3)]
                    t_wsl = P.Ts("wa", 3)
                    wring = Ring(list(zip(wsl, t_wsl)))
                    t_qT = [[P.T("qT%d_%d" % (h, b)) for b in range(4)] for h in range(8)]
                    t_kT = [[P.T("kT%d_%d" % (h, b)) for b in range(4)] for h in range(2)]
                    t_v = P.Ts("vtok", NT)
                    t_tab = P.T("tab")
                    dma("sp", cosS[:], cos_d, writes=[t_tab])
                    dma("sp", sinS[:], sin_d, writes=[t_tab], partial=True)
                    dma("sp", gqk[:, 0:1], gq_d, writes=[t_tab], partial=True)
                    dma("sp", gqk[:, 1:2], gk_d, writes=[t_tab], partial=True)
                    ts("dve", gqk[:, 0:1], gqk[:, 0:1], 128.0 ** -0.5, ALU.mult, reads=[t_tab], writes=[t_tab], partial=True)
                    nw = 2
                    qf = [al2("qf%d" % i, [128, 512], F32) for i in range(nw)]
                    sq = [al2("sq%d" % i, [128, 512], BF16) for i in range(nw)]
                    sd = [al2("sd%d" % i, [128, 512], F32) for i in range(nw)]
                    qn = [al2("qn%d" % i, [128, 512], BF16) for i in range(nw)]
                    t1 = [al2("t1%d" % i, [128, 512], F32) for i in range(1)] * nw
                    t2 = [al2("t2%d" % i, [128, 512], F32) for i in range(1)] * nw
                    t_qf, t_sq, t_sd, t_qn = (P.Ts(n, nw) for n in ("qf", "sq", "sd", "qn"))
                    t_t1 = P.Ts("t1", 1) * nw
                    t_t2 = P.Ts("t2", 1) * nw
                    nrc = [0]
                    accring = Ring([0, 1])
                    auxring = Ring([2, 3])

                    def normrope(bi, out_ap, t_out, gcol, tblk):
                        i = nrc[0] % nw
                        nrc[0] += 1
                        ps = banks[bi]
                        cp("act", qf[i][:], ps[:], reads=[tb[bi]], writes=[t_qf[i]])
                        act(sq[i][:], ps[:], AF.Square, reads=[tb[bi]], writes=[t_sq[i]])
                        b2 = auxring.next()
                        mm(banks[b2][:], onesb[:], sq[i][:], True, True, reads=[t_sq[i], t_c], writes=[tb[b2]])
                        act(sd[i][:], banks[b2][:], AF.Sqrt, reads=[tb[b2]], writes=[t_sd[i]], bias=EPS, scale=1.0 / 128)
                        recip(sd[i][:], sd[i][:], reads=[t_sd[i]], writes=[t_sd[i]])
                        stt(qn[i][:], qf[i][:], gqk[:, gcol:gcol + 1], sd[i][:], ALU.mult, ALU.mult,
                            reads=[t_qf[i], t_sd[i], t_tab], writes=[t_qn[i]])
                        b3 = auxring.next()
                        mm(banks[b3][:], cb[:, PERM, :], qn[i][:], True, True, reads=[t_qn[i], t_c], writes=[tb[b3]])
                        cs = slice(tblk * 512, (tblk + 1) * 512)
                        tt("dve", t1[i][:], qn[i][:], cosS[:, cs], ALU.mult, reads=[t_qn[i], t_tab], writes=[t_t1[i]])
                        tt("dve", t2[i][:], banks[b3][:], sinS[:, cs], ALU.mult, reads=[tb[b3], t_tab], writes=[t_t2[i]])
                        tt("dve", out_ap, t1[i][:], t2[i][:], ALU.add, reads=[t_t1[i], t_t2[i]], writes=[t_out])

                    def fm_block(w, tw, c0, tblk, bi):
                        for k in range(16):
                            mm(banks[bi][:], w[:, k, c0:c0 + 128], xT[:, k, tblk * 512:(tblk + 1) * 512], k == 0, k == 15,
                               reads=[tw] + t_xT[tblk * 4:(tblk + 1) * 4], writes=[tb[bi]])

                    w, tw = wring.next()
                    load_w(w, tw, [(0, w_in[:, 4128:4640])])
                    w2, tw2 = wring.next()
                    load_w(w2, tw2, [(0, w_in[:, 3104:3616])])
                    w3, tw3 = wring.next()
                    load_w(w3, tw3, [(0, w_in[:, 3616:4128])])
                    for h in range(2):
                        for blk in range(4):
                            bi = accring.next()
                            fm_block(w, tw, h * 128, blk, bi)
                            normrope(bi, kT[:, h, blk * 512:(blk + 1) * 512], t_kT[h][blk], 1, blk)
                    for t in range(NT):
                        bi = accring.next()
                        for k in range(16):
                            mm(banks[bi][:, 0:256], xT[:, k, t * 128:(t + 1) * 128], w[:, k, 256:512], k == 0, k == 15,
                               reads=[tw, t_xT[t]], writes=[tb[bi]])
                        cp("act" if t % 2 else "dve", vtok[:, t, :], banks[bi][:, 0:256], reads=[tb[bi]], writes=[t_v[t]])
                    for half, (wq, twq) in enumerate(((w2, tw2), (w3, tw3))):
                        for hh in range(4):
                            h = half * 4 + hh
                            for blk in range(4):
                                bi = accring.next()
                                fm_block(wq, twq, hh * 128, blk, bi)
                                normrope(bi, qT[:, h, blk * 512:(blk + 1) * 512], t_qT[h][blk], 0, blk)
                    if dbg and seq == 0:
                        d_q = dout("dbg_qT", [128, 8, S], BF16)
                        d_k = dout("dbg_kT", [128, 2, S], BF16)
                        d_v = dout("dbg_v", [128, NT, 256], BF16)
                        dma("sp", d_q, qT[:], reads=[t for r in t_qT for t in r])
                        dma("sp", d_k, kT[:], reads=[t for r in t_kT for t in r])
                        dma("sp", d_v, vtok[:], reads=t_v)
                    P.flush()
                    es2.close()
                    NPT = 6
                    pT = [al("pT%d" % i, [128, 512], BF16) for i in range(NPT)]
                    t_pT = P.Ts("pT", NPT)
                    rden = [al("rden%d" % i, [128, 512], F32) for i in range(2)]
                    t_rden = P.Ts("rden", 2)
                    ost = [al("ost%d" % i, [128, 512], BF16) for i in range(2)]
                    t_ost = P.Ts("ost", 2)
                    scring = Ring([0, 1, 2, 3])
                    oring = Ring([(4, 5), (6, 7)])
                    pring = Ring(list(range(NPT)))
                    PF = 3
                    t_HT = P.T("HT")
                    it = 0
                    for h in range(8):
                        g = h // 4
                        for qb in range(4):
                            bo, bd = oring.next()
                            qs = slice(qb * 512, (qb + 1) * 512)

                            def sc(kt):
                                bs = scring.next()
                                mm(banks[bs][:], kT[:, g, kt * 128:(kt + 1) * 128], qT[:, h, qs], True, True,
                                   reads=[t_kT[g][kt // 4], t_qT[h][qb]], writes=[tb[bs]])
                                return bs
                            pend = [sc(kt) for kt in range(PF)]
                            for kt in range(NT):
                                bs = pend.pop(0)
                                pi = pring.next()
                                act(pT[pi][:], banks[bs][:], AF.Exp, reads=[tb[bs]], writes=[t_pT[pi]])
                                if kt + PF < NT:
                                    pend.append(sc(kt + PF))
                                mm(banks[bo][:], vtok[:, kt, g * 128:(g + 1) * 128], pT[pi][:], kt == 0, kt == NT - 1,
                                   reads=[t_v[kt], t_pT[pi]], writes=[tb[bo]])
                                mm(banks[bd][:], onesb[:], pT[pi][:], kt == 0, kt == NT - 1,
                                   reads=[t_c, t_pT[pi]], writes=[tb[bd]])
                            i2 = it % 2
                            it += 1
                            recip(rden[i2][:], banks[bd][:], reads=[tb[bd]], writes=[t_rden[i2]])
                            tt("dve", ost[i2][:], banks[bo][:], rden[i2][:], ALU.mult, reads=[tb[bo], t_rden[i2]],
                               writes=[t_ost[i2]])
                            dma("sp", HT[seq, 1024 + h * 128: 1024 + (h + 1) * 128, qs], ost[i2][:], reads=[t_ost[i2]],
                                writes=[t_HT], partial=True)
                    P.flush()

            if "B" in stages:
                for G in range(2):
                    with ExitStack() as es:
                        al = lambda name, shape, dt: es.enter_context(nc.sbuf_tensor(uq_name(name), shape, dt))
                        qkT = al("qkT", [128, 4, S], BF16)
                        t_qk = [[P.T("qk%d_%d" % (c, t)) for t in range(4)] for c in range(4)]
                        ktok = al("ktok", [128, NT, 256], BF16)
                        t_ktok = P.Ts("ktok", NT)
                        vbase = al("vbase", [128, NT, 4, 129], BF16)
                        t_vb = P.Ts("vb", NT)
                        so = al("so", [128, NT, 512], BF16)
                        t_so = P.Ts("so", NT)
                        gsm = al("gsm", [128, NT, 4, 16], F32)
                        t_gs = P.Ts("gs", NT)
                        cw = al("cw", [128, 4, 5], F32)
                        gbias = al("gbias_sb", [128, 32], F32)
                        gml = al("gml", [128, 512], F32)
                        es2 = ExitStack()
                        al2 = lambda name, shape, dt: es2.enter_context(nc.sbuf_tensor(uq_name(name), shape, dt))
                        wsl = [al2("wb%d" % i, [128, 16, 512], BF16) for i in range(3)]
                        t_wsl = P.Ts("wb", 3)
                        wgt = al2("wgt", [128, 16, 32], BF16)
                        t_wgt = P.T("wgt")
                        t_misc = P.T("miscB")
                        dma("sp", cw[:, 0:2, :], convT[G * 256:(G + 1) * 256, :].rearrange("(c p) k -> p c k", p=128),
                            writes=[t_misc])
                        dma("sp", cw[:, 2:4, :], convT[512 + G * 256:512 + (G + 1) * 256, :].rearrange("(c p) k -> p c k", p=128),
                            writes=[t_misc], partial=True)
                        bcast_row(gbias[:], gbias_d, t_misc)
                        dma("sp", gml[:], gml_d[:, G * 512:(G + 1) * 512].partition_broadcast(128), writes=[t_misc], partial=True)
                        P.op("pool", lambda e: e.memset(vbase[:, :, :, 128:129], 1.0), writes=t_vb)
                        load_w(wsl[0], t_wsl[0], [(0, w_in[:, G * 256:(G + 1) * 256]), (256, w_in[:, 512 + G * 256:512 + (G + 1) * 256])])
                        load_w(wsl[1], t_wsl[1], [(0, w_in[:, 1024 + G * 512:1024 + (G + 1) * 512])])
                        load_w(wsl[2], t_wsl[2], [(0, w_in[:, 2048 + G * 512:2048 + (G + 1) * 512])])
                        load_w(wgt, t_wgt, [(0, w_in[:, 3072:3104])])
                        pc = [al2("pc%d" % i, [128, S + 4], BF16) for i in range(2)]
                        t_pc = P.Ts("pc", 2)
                        cacc = [al2("cacc%d" % i, [128, S], F32) for i in range(1)] * 2
                        t_cacc = P.Ts("cacc", 1) * 2
                        for i in range(2):
                            P.op("pool", lambda e, i=i: e.memset(pc[i][:, 0:2], 0.0), writes=[t_pc[i]])
                            P.op("pool", lambda e, i=i: e.memset(pc[i][:, S + 2:S + 4], 0.0), writes=[t_pc[i]], partial=True)
                        accring = Ring([0, 1, 2, 3])
                        for c in range(4):
                            i = c % 2
                            for blk in range(4):
                                bi = accring.next()
                                for k in range(16):
                                    mm(banks[bi][:], wsl[0][:, k, c * 128:(c + 1) * 128], xT[:, k, blk * 512:(blk + 1) * 512],
                                       k == 0, k == 15, reads=[t_wsl[0]] + t_xT[blk * 4:(blk + 1) * 4], writes=[tb[bi]])
                                cp("act", pc[i][:, 2 + blk * 512: 2 + (blk + 1) * 512], banks[bi][:], reads=[tb[bi]],
                                   writes=[t_pc[i]], partial=True)
                            ts("dve", cacc[i][:], pc[i][:, 0:S], cw[:, c, 0:1], ALU.mult, reads=[t_pc[i], t_misc], writes=[t_cacc[i]])
                            for j in range(1, 5):
                                stt(cacc[i][:], pc[i][:, j:j + S], cw[:, c, j:j + 1], cacc[i][:], ALU.mult, ALU.add,
                                    reads=[t_pc[i], t_misc, t_cacc[i]], writes=[t_cacc[i]])
                            for blk in range(4):
                                act(qkT[:, c, blk * 512:(blk + 1) * 512], cacc[i][:, blk * 512:(blk + 1) * 512], AF.Silu,
                                    reads=[t_cacc[i]], writes=[t_qk[c][blk]])
                        for t in range(NT):
                            bi = accring.next()
                            for pr in range(2):
                                P.op("pe", lambda e, bi=bi, pr=pr, t=t: e.transpose(
                                    bank_bf(bi)[:, pr * 128:(pr + 1) * 128], qkT[:, 2 + pr, t * 128:(t + 1) * 128], cb[:, IDENT, :]),
                                    reads=[t_qk[2 + pr][t // 4], t_c], writes=[tb[bi]], partial=pr > 0)
                            cp("act", ktok[:, t, :], bank_bf(bi)[:, 0:256], reads=[tb[bi]], writes=[t_ktok[t]])
                        zt = [al2("zt%d" % i, [128, 32], F32) for i in range(2)]
                        lf = [al2("lf%d" % i, [128, 16], F32) for i in range(2)]
                        eb = [al2("eb%d" % i, [128, 16], F32) for i in range(2)]
                        d1 = [al2("d1%d" % i, [128, 16], F32) for i in range(2)]
                        t_zt, t_lf, t_eb, t_d1 = (P.Ts(n, 2) for n in ("zt", "lf", "eb", "d1"))
                        for t in range(NT):
                            i = t % 2
                            xs_ = slice(t * 128, (t + 1) * 128)
                            bi = accring.next()
                            for k in range(16):
                                mm(banks[bi][:], xT[:, k, xs_], wsl[1][:, k, :], k == 0, k == 15, reads=[t_wsl[1], t_xT[t]],
                                   writes=[tb[bi]])
                            cp("act", vbase[:, t, :, 0:128], banks[bi][:].rearrange("p (a b) -> p a b", a=4), reads=[tb[bi]],
                               writes=[t_vb[t]], partial=True)
                            bi = accring.next()
                            for k in range(16):
                                mm(banks[bi][:], xT[:, k, xs_], wsl[2][:, k, :], k == 0, k == 15, reads=[t_wsl[2], t_xT[t]],
                                   writes=[tb[bi]])
                            act(so[:, t, :], banks[bi][:], AF.Sigmoid, reads=[tb[bi]], writes=[t_so[t]])
                            bi = accring.next()
                            for k in range(16):
                                mm(banks[bi][:, 0:32], xT[:, k, xs_], wgt[:, k, :], k == 0, k == 15, reads=[t_wgt, t_xT[t]],
                                   writes=[tb[bi]])
                            tt("dve", zt[i][:], banks[bi][:, 0:32], gbias[:], ALU.add, reads=[tb[bi], t_misc], writes=[t_zt[i]])
                            z4 = zt[i][:].rearrange("p (a b c) -> p a b c", a=2, b=2)
                            lf3 = lf[i][:].rearrange("p (a c) -> p a c", a=2)
                            act(lf3, z4[:, :, 1, :], AF.Exp, reads=[t_zt[i]], writes=[t_lf[i]], scale=-1.0)
                            act(lf[i][:], lf[i][:], AF.Ln, reads=[t_lf[i]], writes=[t_lf[i]], bias=1.0)
                            ts("dve", lf[i][:], lf[i][:], -1.0, ALU.mult, reads=[t_lf[i]], writes=[t_lf[i]])
                            bi = accring.next()
                            mm(banks[bi][:, 0:8], cf[:, MASKF, :], lf[i][:, 0:8], True, True, reads=[t_lf[i], t_c], writes=[tb[bi]])
                            mm(banks[bi][:, 8:16], cf[:, MASKB, :], lf[i][:, 8:16], True, True, reads=[t_lf[i], t_c],
                               writes=[tb[bi]], )
                            act(eb[i][:], banks[bi][:, 0:16], AF.Exp, reads=[tb[bi]], writes=[t_eb[i]])
                            ts("dve", gsm[:, t, 0, :], eb[i][:], 0.125, ALU.mult, reads=[t_eb[i]], writes=[t_gs[t]])
                            tt("dve", d1[i][:].rearrange("p (a c) -> p a c", a=2), z4[:, :, 0, :],
                               banks[bi][:, 0:16].rearrange("p (a c) -> p a c", a=2), ALU.subtract, reads=[t_zt[i], tb[bi]],
                               writes=[t_d1[i]])
                            act(gsm[:, t, 1, :], d1[i][:], AF.Exp, reads=[t_d1[i]], writes=[t_gs[t]], partial=True)
                            bi = accring.next()
                            mm(banks[bi][:, 0:8], cf[:, SELL, :], eb[i][:, 0:8], True, True, reads=[t_eb[i], t_c], writes=[tb[bi]])
                            mm(banks[bi][:, 8:16], cf[:, SELF, :], eb[i][:, 8:16], True, True, reads=[t_eb[i], t_c], writes=[tb[bi]])
                            cp("act", gsm[:, t, 3, :], banks[bi][:, 0:16], reads=[tb[bi]], writes=[t_gs[t]], partial=True)
                            tt("dve", gsm[:, t, 2, :], gsm[:, t, 1, :], banks[bi][:, 0:16], ALU.mult, reads=[tb[bi], t_gs[t]],
                               writes=[t_gs[t]], partial=True)
                        if dbg and seq == 0 and G == 0:
                            d_qk = dout("dbg_qk", [128, 4, S], BF16)
                            d_gs = dout("dbg_gs", [128, NT, 4, 16], F32)
                            d_kt = dout("dbg_ktok", [128, NT, 256], BF16)
                            dma("sp", d_qk, qkT[:], reads=[t for r in t_qk for t in r])
                            dma("sp", d_gs, gsm[:], reads=t_gs)
                            dma("sp", d_kt, ktok[:], reads=t_ktok)
                        P.flush()
                        es2.close()
                        hpart = al("hpart", [128, NT, 4, 128], BF16)
                        t_hp = [[P.T("hp%d_%d" % (t, h)) for h in range(4)] for t in range(NT)]
                        Cf = al("Cf", [128, 2, 2, 129], F32)
                        Cb = al("Cb", [128, 2, 2, 129], BF16)
                        t_C = [[[P.T("C%d%d%d" % (a, b, c)) for c in range(2)] for b in range(2)] for a in range(2)]
                        t_Cb = [[[P.T("Cb%d%d%d" % (a, b, c)) for c in range(2)] for b in range(2)] for a in range(2)]
                        P.op("dve", lambda e: e.memset(Cf[:], 0.0), writes=[t for a in t_C for b in a for t in b])
                        P.op("pool", lambda e: e.memset(Cb[:], 0.0), writes=[t for a in t_Cb for b in a for t in b])
                        NR = 4
                        sm = [al("sm%d" % i, [128, 128], BF16) for i in range(NR)]
                        v1 = [al("v1%d" % i, [128, 129], BF16) for i in range(NR)]
                        v2 = [al("v2%d" % i, [128, 129], BF16) for i in range(NR)]
                        dd = [al("dd%d" % i, [128, 2], F32) for i in range(NR)]
                        hs = [al("hs%d" % i, [128, 128], F32) for i in range(NR)]
                        hj = [al("hj%d" % i, [128, 128], F32) for i in range(NR)]
                        ssq = [al("ssq%d" % i, [128, 2], F32) for i in range(NR)]
                        t_sm, t_v1, t_v2, t_dd, t_hs, t_hj, t_ssq = (P.Ts(n, NR) for n in ("sm", "v1", "v2", "dd", "hs", "hj", "ssq"))
                        htok = [al("htok%d" % i, [128, 4, 128], BF16) for i in range(2)]
                        t_htok = P.Ts("htok", 2)
                        hTst = [al("hTst%d" % i, [128, 4, 128], BF16) for i in range(2)]
                        t_hTst = P.Ts("hTst", 2)
                        t_HT = P.T("HTb")
                        stq = Ring([0, 1])
                        oq = Ring([2, 3, 4])
                        uq = Ring([5, 6])
                        u = 0
                        done = [0] * NT
                        nfin = 0
                        LAG = 2
                        ulist = []
                        for step in range(NT):
                            for dr in range(2):
                                c = step if dr == 0 else NT - 1 - step
                                for hl in range(4):
                                    ulist.append((dr, c, hl))
                        ctx = {}
                        state = {"nfin": 0}

                        def front(uidx):
                            dr, c, hl = ulist[uidx]
                            cs = slice(c * 128, (c + 1) * 128)
                            pr, ho = hl // 2, 64 * (hl % 2)
                            rows = slice(ho, ho + 64)
                            col = dr * 8 + G * 4 + hl
                            i = uidx % NR
                            b_st = stq.next()
                            mm(banks[b_st][:, 0:128], qkT[rows, 2 + pr, cs], qkT[rows, pr, cs], True, True,
                               reads=[t_qk[2 + pr][c // 4], t_qk[pr][c // 4]], writes=[tb[b_st]])
                            tt("dve", sm[i][:], banks[b_st][:, 0:128], cf[:, MASKF if dr == 0 else MASKB, :], ALU.mult,
                               reads=[tb[b_st], t_c], writes=[t_sm[i]])
                            act(v1[i][:], vbase[:, c, hl, :], AF.Copy, reads=[t_vb[c], t_gs[c]], writes=[t_v1[i]],
                                scale=gsm[:, c, 1, col:col + 1])
                            ts("dve", v2[i][:], vbase[:, c, hl, :], gsm[:, c, 2, col:col + 1], ALU.mult,
                               reads=[t_vb[c], t_gs[c]], writes=[t_v2[i]])

                        def front_b(uidx):
                            dr, c, hl = ulist[uidx]
                            cs = slice(c * 128, (c + 1) * 128)
                            pr, ho = hl // 2, 64 * (hl % 2)
                            rows = slice(ho, ho + 64)
                            col = dr * 8 + G * 4 + hl
                            i = uidx % NR
                            b_o = oq.next()
                            mm(banks[b_o][:, 0:129], sm[i][:], v1[i][:], True, False, reads=[t_sm[i], t_v1[i]],
                               writes=[tb[b_o]])
                            mm(banks[b_o][:, 0:129], qkT[rows, pr, cs], Cb[rows, pr, dr, :], False, True,
                               reads=[t_qk[pr][c // 4], t_Cb[pr][dr][hl % 2]], writes=[tb[b_o]])
                            b_u = uq.next()
                            mm(banks[b_u][:, 0:129], ktok[:, c, pr * 128:(pr + 1) * 128], v2[i][:], True, True,
                               reads=[t_ktok[c], t_v2[i]], writes=[tb[b_u]])
                            tC = t_C[pr][dr][hl % 2]
                            stt(Cf[rows, pr, dr, :], Cf[rows, pr, dr, :], gsm[rows, c, 3, col:col + 1], banks[b_u][rows, 0:129],
                                ALU.mult, ALU.add, reads=[tC, t_gs[c], tb[b_u]], writes=[tC])
                            cp("act", Cb[rows, pr, dr, :], Cf[rows, pr, dr, :], reads=[tC], writes=[t_Cb[pr][dr][hl % 2]])
                            ctx[uidx] = b_o

                        def back(uidx):
                            dr, c, hl = ulist[uidx]
                            cs = slice(c * 128, (c + 1) * 128)
                            col = dr * 8 + G * 4 + hl
                            i = uidx % NR
                            b_o = ctx.pop(uidx)
                            act(dd[i][:, 0:1], banks[b_o][:, 128:129], AF.Abs, reads=[tb[b_o], t_gs[c]], writes=[t_dd[i]],
                                scale=gsm[:, c, 0, col:col + 1])
                            ts("dve", dd[i][:, 0:1], dd[i][:, 0:1], 1.0, ALU.max, reads=[t_dd[i]], writes=[t_dd[i]])
                            recip(dd[i][:, 0:1], dd[i][:, 0:1], reads=[t_dd[i]], writes=[t_dd[i]])
                            ts("dve", dd[i][:, 1:2], dd[i][:, 0:1], gsm[:, c, 0, col:col + 1], ALU.mult,
                               reads=[t_dd[i], t_gs[c]], writes=[t_dd[i]])
                            if done[c] < 4:
                                act(hpart[:, c, hl, :], banks[b_o][:, 0:128], AF.Copy, reads=[tb[b_o], t_dd[i]],
                                    writes=[t_hp[c][hl]], scale=dd[i][:, 1:2])
                            else:
                                stt(hs[i][:], banks[b_o][:, 0:128], dd[i][:, 1:2], hpart[:, c, hl, :], ALU.mult, ALU.add,
                                    reads=[tb[b_o], t_dd[i], t_hp[c][hl]], writes=[t_hs[i]])
                                act(hj[i][:], hs[i][:], AF.Square, reads=[t_hs[i]], writes=[t_hj[i], t_ssq[i]],
                                    accum_out=ssq[i][:, 0:1])
                                act(ssq[i][:, 1:2], ssq[i][:, 0:1], AF.Sqrt, reads=[t_ssq[i]], writes=[t_ssq[i]],
                                    bias=EPS, scale=1.0 / 128)
                                recip(ssq[i][:, 1:2], ssq[i][:, 1:2], reads=[t_ssq[i]], writes=[t_ssq[i]])
                                stt(hj[i][:], hs[i][:], ssq[i][:, 1:2], gml[:, hl * 128:(hl + 1) * 128], ALU.mult, ALU.mult,
                                    reads=[t_hs[i], t_ssq[i], t_misc], writes=[t_hj[i]])
                                f2 = state["nfin"] % 2
                                tt("dve", htok[f2][:, hl, :], hj[i][:], so[:, c, hl * 128:(hl + 1) * 128], ALU.mult,
                                   reads=[t_hj[i], t_so[c]], writes=[t_htok[f2]], partial=hl > 0)
                                if hl == 3:
                                    b_t = uq.next()
                                    for j in range(4):
                                        P.op("pe", lambda e, b_t=b_t, j=j, f2=f2: e.transpose(
                                            bank_bf(b_t)[:, j * 128:(j + 1) * 128], htok[f2][:, j, :], cb[:, IDENT, :]),
                                            reads=[t_htok[f2], t_c], writes=[tb[b_t]], partial=j > 0)
                                    cp("act", hTst[f2][:], bank_bf(b_t)[:, 0:512].rearrange("p (a b) -> p a b", a=4),
                                       reads=[tb[b_t]], writes=[t_hTst[f2]])
                                    dma("sp", HT[seq, G * 512:(G + 1) * 512, cs].rearrange("(a p) t -> p a t", p=128),
                                        hTst[f2][:], reads=[t_hTst[f2]], writes=[t_HT], partial=True)
                                    state["nfin"] += 1
                            done[c] += 1

                        nun = len(ulist) if "rec" not in SKIP else 0
                        if nun:
                            front(0)
                        for k in range(nun + LAG if nun else 0):
                            if k + 1 < nun:
                                front(k + 1)
                            if k < nun:
                                front_b(k)
                            if k - LAG >= 0:
                                back(k - LAG)
                        P.flush()

        if "C" in stages:
            with ExitStack() as es:
                al = lambda name, shape, dt: es.enter_context(nc.sbuf_tensor(uq_name(name), shape, dt))
                wo = al("wo", [128, 16, D], BF16)
                wpg = al("wpg", [128, 16, D], BF16)
                wpp = al("wpp", [128, 2, D], BF16)
                wr = al("wr", [128, 16, NEXP], F32)
                lng = al("lng", [128, D], F32)
                lnb = al("lnb", [128, D], F32)
                bpgb = al("bpgb", [1, D], BF16)
                t_wo, t_wpg, t_wpp, t_wr, t_ln = P.T("wo"), P.T("wpg"), P.T("wpp"), P.T("wr"), P.T("ln1")
                for hf in range(4):
                    cs = slice(hf * 512, (hf + 1) * 512)
                    dma("pool", wo[:, :, cs], w_out[:, cs].rearrange("(c p) f -> p c f", p=128), writes=[t_wo], partial=hf > 0)
                for hf in range(4):
                    cs = slice(hf * 512, (hf + 1) * 512)
                    dma("pool", wpg[:, :, cs], wpg_d[:, cs].rearrange("(c p) f -> p c f", p=128), writes=[t_wpg], partial=hf > 0)
                dma("pool", wpp[:], wpp_d.rearrange("(c p) f -> p c f", p=128), writes=[t_wpp])
                if "bpgb" not in SKIP:
                    dma("pool", bpgb[:], bpg_d, writes=[t_wpp], partial=True)
                dma("sp", wr[:], wr_d.rearrange("(c p) f -> p c f", p=128), writes=[t_wr])
                dma("sp", lng[:], ln1g_d.partition_broadcast(128), writes=[t_ln])
                dma("sp", lnb[:], ln1b_d.partition_broadcast(128), writes=[t_ln], partial=True)
                hTt = al("hTt", [128, 16, 128], BF16)
                xt = al("xt", [128, D], F32)
                pt = al("pt", [128, 256], F32)
                x1b = al("x1b", [128, D], BF16)
                x1Tf = al("x1Tf", [128, 16, 128], F32)
                x1Tb = al("x1Tb", [128, 16, 128], BF16)
                pTb = al("pTb", [128, 2, 128], BF16)
                sg = al("sg", [128, 512], F32)
                plw = al("plw", [128, 512], F32)
                lnw = al("lnw", [128, 32], F32)
                rt = al("rt", [128, 24], F32)
                affp = al("affp", [128, 64], F32)
                t_hTt, t_xt, t_pt, t_x1b, t_x1Tf, t_x1Tb, t_pTb, t_sg, t_plw, t_lnw, t_rt, t_affp = (
                    P.T(n) for n in ("hTt", "xt", "pt", "x1b", "x1Tf", "x1Tb", "pTb", "sg", "plw", "lnw", "rt", "affp"))
                t_X1B, t_R = P.T("X1B"), P.T("Rw")
                P.op("dve", lambda e: e.memset(affp[:], 0.0), writes=[t_affp])
                bring = Ring(list(range(8)))
                for t in range(NT):
                    rows = slice(tok0 + t * 128, tok0 + (t + 1) * 128)
                    dma("sp", hTt[:], HT[seq].rearrange("(c p) t -> p c t", p=128)[:, :, t * 128:(t + 1) * 128], writes=[t_hTt])
                    dma("sp", xt[:], x_d[rows, :], writes=[t_xt])
                    dma("sp", pt[:], p_d[rows, :], writes=[t_pt])
                    for dg in range(4):
                        bi = bring.next()
                        cs = slice(dg * 512, (dg + 1) * 512)
                        for k in range(16):
                            mm(banks[bi][:], hTt[:, k, :], wo[:, k, cs], k == 0, k == 15, reads=[t_hTt, t_wo], writes=[tb[bi]])
                        stt(xt[:, cs], xt[:, cs], ALPHA, banks[bi][:], ALU.mult, ALU.add, reads=[t_xt, tb[bi]], writes=[t_xt],
                            partial=dg > 0)
                    if "ln" not in SKIP:
                        layernorm(xt, t_xt, lng, lnb, t_ln, lnw, t_lnw)
                    if "x1b" not in SKIP:
                        cp("act", x1b[:], xt[:], reads=[t_xt], writes=[t_x1b])
                        dma("sp", X1B[rows, :], x1b[:], reads=[t_x1b], writes=[t_X1B], partial=True)
                    for g in range(4 if "xtr" not in SKIP else 0):
                        bi = bring.next()
                        for j in range(4):
                            c = g * 4 + j
                            P.op("pe", lambda e, bi=bi, j=j, c=c: e.transpose(
                                banks[bi][:, j * 128:(j + 1) * 128], xt[:, c * 128:(c + 1) * 128], cf[:, IDENT, :]),
                                reads=[t_xt, t_c], writes=[tb[bi]], partial=j > 0)
                        pv = banks[bi][:].rearrange("p (a b) -> p a b", a=4)
                        cp("act", x1Tf[:, g * 4:(g + 1) * 4, :], pv, reads=[tb[bi]], writes=[t_x1Tf], partial=g > 0)
                        if "x1tb" not in SKIP:
                            cp("dve", x1Tb[:, g * 4:(g + 1) * 4, :], x1Tf[:, g * 4:(g + 1) * 4, :], reads=[t_x1Tf], writes=[t_x1Tb],
                               partial=g > 0)
                    if "router" not in SKIP:
                        bi = bring.next()
                        for k in range(16):
                            mm(banks[bi][:, 0:NEXP], x1Tf[:, k, :], wr[:, k, :], k == 0, k == 15, reads=[t_x1Tf, t_wr], writes=[tb[bi]])
                        P.op("dve", lambda e, bi=bi: e.tensor_reduce(out=rt[:, 16:17], in_=banks[bi][:, 0:NEXP],
                                                                      axis=mybir.AxisListType.X, op=ALU.max, negate=True),
                             reads=[tb[bi]], writes=[t_rt])
                        act(rt[:, 0:NEXP], banks[bi][:, 0:NEXP], AF.Exp, reads=[tb[bi], t_rt], writes=[t_rt], bias=rt[:, 16:17],
                            accum_out=rt[:, 17:18])
                        recip(rt[:, 18:19], rt[:, 17:18], reads=[t_rt], writes=[t_rt])
                        ts("dve", affp[:, 32 * seq:32 * seq + NEXP], rt[:, 0:NEXP], rt[:, 18:19], ALU.mult, reads=[t_rt],
                           writes=[t_affp])
                        bi = bring.next()
                        mm(banks[bi][0:64, 0:128], affp[:], cf[:, IDENT, :], True, True, reads=[t_affp, t_c], writes=[tb[bi]])
                        cp("act", affT[32 * seq:32 * seq + NEXP, t * 128:(t + 1) * 128], banks[bi][32 * seq:32 * seq + NEXP, 0:128],
                           reads=[tb[bi]], writes=[t_affT], partial=True)
                    if "pl" not in SKIP:
                        bi = bring.next()
                        for j in range(2):
                            P.op("pe", lambda e, bi=bi, j=j: e.transpose(
                                banks[bi][:, j * 128:(j + 1) * 128], pt[:, j * 128:(j + 1) * 128], cf[:, IDENT, :]),
                                reads=[t_pt, t_c], writes=[tb[bi]], partial=j > 0)
                        cp("act", pTb[:], banks[bi][:, 0:256].rearrange("p (a b) -> p a b", a=2), reads=[tb[bi]], writes=[t_pTb])
                        for dg in range(4):
                            cs = slice(dg * 512, (dg + 1) * 512)
                            bi = bring.next()
                            for k in range(16):
                                mm(banks[bi][:], x1Tb[:, k, :], wpg[:, k, cs], k == 0, False, reads=[t_x1Tb, t_wpg], writes=[tb[bi]])
                            mm(banks[bi][:], onesb[0:1, :], bpgb[0:1, cs], False, True, reads=[t_c, t_wpp], writes=[tb[bi]])
                            act(sg[:], banks[bi][:], AF.Sigmoid, reads=[tb[bi]], writes=[t_sg])
                            b2 = bring.next()
                            for k in range(2):
                                mm(banks[b2][:], pTb[:, k, :], wpp[:, k, cs], k == 0, k == 1, reads=[t_pTb, t_wpp], writes=[tb[b2]])
                            tt("dve", plw[:], sg[:], banks[b2][:], ALU.mult, reads=[t_sg, tb[b2]], writes=[t_plw])
                            stt(xt[:, cs], xt[:, cs], ALPHA, plw[:], ALU.mult, ALU.add, reads=[t_xt, t_plw], writes=[t_xt])
                    dma("sp", R[rows, :], xt[:], reads=[t_xt], writes=[t_R], partial=True)
                P.flush()

    if "M" in stages:
        with ExitStack() as es:
            al = lambda name, shape, dt: es.enter_context(nc.sbuf_tensor(uq_name(name), shape, dt))
            NC_ = 2 * nseq
            NCOL = 128 * NC_
            tkw = al("tkw", [64, S], F32)
            top = al("top", [64, CAP], F32)
            idxu = al("idxu", [64, CAP], U32)
            idxf = al("idxf", [64, CAP], F32)
            idxT = al("idxT", [128, 2, 64], I32)
            gateT = al("gateT", [128, 2, 64], F32)
            t_tk, t_top, t_idxu, t_idxf, t_idxT, t_gateT = (P.T(n) for n in ("tkw", "top", "idxu", "idxf", "idxT", "gateT"))
            cp("dve", tkw[:], affT[:], reads=[t_affT], writes=[t_tk])
            for r in range(CAP // 8):
                rs = slice(r * 8, (r + 1) * 8)
                P.op("dve", lambda e, rs=rs: e.max(out=top[:, rs], in_=tkw[:]), reads=[t_tk], writes=[t_top], partial=True)
                P.op("dve", lambda e, rs=rs: e.max_index(out=idxu[:, rs], in_max=top[:, rs], in_values=tkw[:]),
                     reads=[t_tk, t_top], writes=[t_idxu], partial=True)
                P.op("dve", lambda e, rs=rs: e.match_replace(out=tkw[:], in_to_replace=top[:, rs], in_values=tkw[:], imm_value=-1.0),
                     reads=[t_top, t_tk], writes=[t_tk])
            cp("dve", idxf[:], idxu[:], reads=[t_idxu], writes=[t_idxf])
            if nseq > 1:
                ts("dve", idxf[32:48, :], idxf[32:48, :], float(S), ALU.add, reads=[t_idxf], writes=[t_idxf])
            for ct in range(2):
                P.op("pe", lambda e, ct=ct: e.transpose(banks[0][:, ct * 64:(ct + 1) * 64], idxf[:, ct * 128:(ct + 1) * 128],
                                                      cf[0:64, IDENT, 0:64]), reads=[t_idxf, t_c], writes=[tb[0]], partial=ct > 0)
                P.op("pe", lambda e, ct=ct: e.transpose(banks[1][:, ct * 64:(ct + 1) * 64], top[:, ct * 128:(ct + 1) * 128],
                                                      cf[0:64, IDENT, 0:64]), reads=[t_top, t_c], writes=[tb[1]], partial=ct > 0)
            cp("dve", idxT[:], banks[0][:, 0:128].rearrange("p (a b) -> p a b", a=2), reads=[tb[0]], writes=[t_idxT])
            cp("act", gateT[:], banks[1][:, 0:128].rearrange("p (a b) -> p a b", a=2), reads=[tb[1]], writes=[t_gateT])
            if dbg:
                d_idx = dout("dbg_idxT", [128, 2, 64], I32)
                d_gate = dout("dbg_gateT", [128, 2, 64], F32)
                dma("sp", d_idx, idxT[:], reads=[t_idxT])
                dma("sp", d_gate, gateT[:], reads=[t_gateT])
            NSL = 7
            wsl = [al("wm%d" % i, [128, 8192], BF16) for i in range(NSL)]
            t_wsl = P.Ts("wm", NSL)
            xs = [al("xs%d" % i, [128, D], BF16) for i in range(NC_)]
            t_xs = P.Ts("xs", NC_)
            xsT = al("xsT", [128, 16, NCOL], BF16)
            t_xsT = P.Ts("xsT", NC_)
            hT = al("hT", [128, 8, NCOL], BF16)
            t_hT = P.Ts("hT", 8)
            sgw = [al("sgw%d" % i, [128, NCOL], F32) for i in range(2)]
            t_sgw = P.Ts("sgw", 2)
            yst = [al("yst%d" % i, [128, D], F32) for i in range(2)]
            t_yst = P.Ts("yst", 2)
            t_Rm = P.T("Rm")
            t_X1Bm = P.T("X1Bm")

            def piece_ap(n):
                e, i = divmod(n, 6)
                if i < 4:
                    src = (wg_d if i % 2 == 0 else wu_d)[e][:, (i // 2) * 512:(i // 2 + 1) * 512]
                    return src.rearrange("(c p) f -> p c f", p=128), "p (k f) -> p k f", 16
                hh = i - 4
                return wd_d[e][:, hh * 1024:(hh + 1) * 1024].rearrange("(c p) f -> p c f", p=128), "p (k f) -> p k f", 8

            def load_piece(n):
                if n >= 6 * NEXP:
                    return
                src, pat, kk = piece_ap(n)
                sl = n % NSL
                dma("pool", wsl[sl][:].rearrange(pat, k=kk), src, writes=[t_wsl[sl]])

            def wview(n, kk):
                return wsl[n % NSL][:].rearrange("p (k f) -> p k f", k=kk), t_wsl[n % NSL]

            for n in range(NSL):
                load_piece(n)
            bring = Ring(list(range(8)))
            sgi = 0
            for e in range(NEXP):
                cols = []
                for j in range(NC_):
                    sq_, ct = divmod(j, 2)
                    col = 32 * sq_ + e
                    cols.append((ct, col))
                    P.op("pool", lambda eng, j=j, ct=ct, col=col: eng.indirect_dma_start(
                        out=xs[j][:], out_offset=None, in_=X1B,
                        in_offset=bass.IndirectOffsetOnAxis(ap=idxT[:, ct, col:col + 1], axis=0)),
                        reads=[t_idxT, t_X1Bm], writes=[t_xs[j]], dma=True)
                for j in range(NC_):
                    for g4 in range(4):
                        bi = bring.next()
                        for q4 in range(4):
                            c = g4 * 4 + q4
                            P.op("pe", lambda eng, bi=bi, q4=q4, c=c, j=j: eng.transpose(
                                bank_bf(bi)[:, q4 * 128:(q4 + 1) * 128], xs[j][:, c * 128:(c + 1) * 128], cb[:, IDENT, :]),
                                reads=[t_xs[j], t_c], writes=[tb[bi]], partial=q4 > 0)
                        cp("act" if g4 % 2 else "dve", xsT[:, g4 * 4:(g4 + 1) * 4, j * 128:(j + 1) * 128],
                           bank_bf(bi)[:, 0:512].rearrange("p (a b) -> p a b", a=4), reads=[tb[bi]], writes=[t_xsT[j]],
                           partial=g4 > 0)
                for fh in range(2):
                    ng, nu = 6 * e + 2 * fh, 6 * e + 2 * fh + 1
                    wgv, twg = wview(ng, 16)
                    wuv, twu = wview(nu, 16)
                    for ft in range(4):
                        f = fh * 4 + ft
                        bg = bring.next()
                        for k in range(16):
                            mm(banks[bg][:, 0:NCOL], wgv[:, k, ft * 128:(ft + 1) * 128], xsT[:, k, :], k == 0, k == 15,
                               reads=[twg] + t_xsT, writes=[tb[bg]])
                        bu = bring.next()
                        for k in range(16):
                            mm(banks[bu][:, 0:NCOL], wuv[:, k, ft * 128:(ft + 1) * 128], xsT[:, k, :], k == 0, k == 15,
                               reads=[twu] + t_xsT, writes=[tb[bu]])
                        si = sgi % 2
                        sgi += 1
                        act(sgw[si][:], banks[bg][:, 0:NCOL], AF.Silu, reads=[tb[bg]], writes=[t_sgw[si]])
                        tt("dve", hT[:, f, :], sgw[si][:], banks[bu][:, 0:NCOL], ALU.mult, reads=[t_sgw[si], tb[bu]],
                           writes=[t_hT[f]])
                    load_piece(ng + NSL)
                    load_piece(nu + NSL)
                nd0, nd1 = 6 * e + 4, 6 * e + 5
                for j in range(NC_):
                    ct, col = cols[j]
                    yi = (e * NC_ + j) % 2
                    for dh, nd in enumerate((nd0, nd1)):
                        wdv, twd = wview(nd, 8)
                        for dg2 in range(2):
                            bi = bring.next()
                            for k in range(8):
                                mm(banks[bi][:], hT[:, k, j * 128:(j + 1) * 128], wdv[:, k, dg2 * 512:(dg2 + 1) * 512], k == 0, k == 7,
                                   reads=[t_hT[k], twd], writes=[tb[bi]])
                            oc = dh * 1024 + dg2 * 512
                            act(yst[yi][:, oc:oc + 512], banks[bi][:], AF.Copy, reads=[tb[bi], t_gateT], writes=[t_yst[yi]],
                                scale=gateT[:, ct, col:col + 1], partial=(dh + dg2) > 0)
                    P.op("pool", lambda eng, yi=yi, ct=ct, col=col: eng.indirect_dma_start(
                        out=R, out_offset=bass.IndirectOffsetOnAxis(ap=idxT[:, ct, col:col + 1], axis=0),
                        in_=yst[yi][:], in_offset=None, compute_op=ALU.add),
                        reads=[t_idxT, t_yst[yi]], writes=[t_Rm], dma=True)
                load_piece(nd0 + NSL)
                load_piece(nd1 + NSL)
            P.flush()

    if "F" in stages:
        with ExitStack() as es:
            al = lambda name, shape, dt: es.enter_context(nc.sbuf_tensor(uq_name(name), shape, dt))
            lng = al("ln2g", [128, D], F32)
            lnb = al("ln2b", [128, D], F32)
            t_ln = P.T("ln2")
            dma("sp", lng[:], ln2g_d.partition_broadcast(128), writes=[t_ln])
            dma("sp", lnb[:], ln2b_d.partition_broadcast(128), writes=[t_ln], partial=True)
            NB = 4
            rt_ = [al("rf%d" % i, [128, D], F32) for i in range(NB)]
            lw = [al("lw%d" % i, [128, 32], F32) for i in range(NB)]
            t_rt_ = P.Ts("rf", NB)
            t_lw = P.Ts("lw", NB)
            t_out = P.T("out")
            for t in range(nseq * NT):
                i = t % NB
                rows = slice(t * 128, (t + 1) * 128)
                dma("sp", rt_[i][:], R[rows, :], writes=[t_rt_[i]])
                layernorm(rt_[i], t_rt_[i], lng, lnb, t_ln, lw[i], t_lw[i])
                dma("sp", out_d[rows, :], rt_[i][:], reads=[t_rt_[i]], writes=[t_out], partial=True)
    P.flush(final=True)
    return nc, dbg_out, P


def _consts():
    c = np.zeros((6, 128, 128), np.float32)
    i = np.arange(128)
    c[0] = np.eye(128, dtype=np.float32)
    c[1] = (i[:, None] <= i[None, :]).astype(np.float32)
    c[2] = (i[:, None] >= i[None, :]).astype(np.float32)
    c[3][127, :] = 1.0
    c[4][0, :] = 1.0
    partner = np.where((i % 64) < 32, i + 32, i - 32)
    c[5][partner, i] = 1.0
    t = np.arange(S)
    row = (t // 64).astype(np.float32)
    colp = (t % 64).astype(np.float32)
    inv = (np.float32(10000.0) ** (-np.arange(32, dtype=np.float32) / np.float32(32))).astype(np.float32)
    cosT = np.zeros((128, S), np.float32)
    sinT = np.zeros((128, S), np.float32)
    for d in range(128):
        pos = row if d < 64 else colp
        ang = (pos * inv[d % 32]).astype(np.float32)
        cosT[d] = np.cos(ang)
        sgn = -1.0 if (d % 64) < 32 else 1.0
        sinT[d] = sgn * np.sin(ang)
    return c, cosT, sinT


def prep_shared(inp):
    f = lambda a: np.ascontiguousarray(np.asarray(a, dtype=np.float32))
    c, cosT, sinT = _consts()
    bi = f(inp["b_igate"])[0]
    bf = f(inp["b_fgate"])[0]
    gb = np.stack([bi, bf], axis=1).reshape(1, 32)
    return {
        "w_in": f(inp["w_in"])[0], "convT": f(np.asarray(inp["conv_w"])[0].T), "gbias": f(gb),
        "g_mlstm": f(inp["g_mlstm"]).reshape(1, 1024), "g_q": f(inp["g_q"]).reshape(128, 1),
        "g_k": f(inp["g_k"]).reshape(128, 1), "w_out": f(inp["w_out"])[0],
        "ln1_g": f(inp["ln1_g"]).reshape(1, D), "ln1_b": f(inp["ln1_b"]).reshape(1, D),
        "w_router": f(inp["w_router"])[0], "w_gate": f(inp["w_gate"])[0], "w_up": f(inp["w_up"])[0],
        "w_down": f(inp["w_down"])[0], "w_pl_proj": f(inp["w_pl_proj"])[0], "w_pl_gate": f(inp["w_pl_gate"])[0],
        "b_pl_gate": f(inp["b_pl_gate"]).reshape(1, D), "ln2_g": f(inp["ln2_g"]).reshape(1, D),
        "ln2_b": f(inp["ln2_b"]).reshape(1, D), "cst": c, "cosT": cosT, "sinT": sinT,
    }


_CACHE = {}


def kernel(**inputs):
    ncores = 8
    sh = prep_shared(inputs)
    x = np.asarray(inputs["x"], dtype=np.float32)
    p = np.asarray(inputs["p"], dtype=np.float32)[0]
    nseq = x.shape[0] // ncores
    if "nc" not in _CACHE:
        _CACHE["nc"] = build(nseq=nseq, stages="ABCMF", dbg=False)[0]
    nc = _CACHE["nc"]
    in_maps = []
    for c in range(ncores):
        m = dict(sh)
        m["x"] = np.ascontiguousarray(x[c * nseq:(c + 1) * nseq].reshape(nseq * S, D))
        m["p"] = np.ascontiguousarray(p[c * nseq:(c + 1) * nseq].reshape(nseq * S, 256))
        in_maps.append(m)
    res = run_bass_kernel_spmd(nc, in_maps, core_ids=list(range(ncores)))
    out = np.concatenate([np.asarray(r["out"]).reshape(nseq, S, D) for r in res.results], axis=0)
    return out.astype(np.float32)
```
